# Optimizing a Trainium2 kernel written in Bass

```python
import jax, jax.numpy as jnp
from jax import lax
import numpy as np

D_MODEL = 2048
BATCH = 2
SEQ = 8192
DEPTH = 4

N_MIXERS = 4
Q_BLOCK = 128
EPS = 1e-6
FOX_HEADS = 16
FOX_HEAD_DIM = D_MODEL // FOX_HEADS
LRU_WIDTH = 2688
LRU_BLOCKS = 16
LRU_BLOCK_DIM = LRU_WIDTH // LRU_BLOCKS
CONV_WIDTH = 4
LRU_C = 8.0
SB_HEADS = 16
SB_HEAD_DIM = D_MODEL // SB_HEADS
MLSTM_HEADS = 4
MLSTM_QK_DIM = D_MODEL // 2 // MLSTM_HEADS
MLSTM_V_DIM = D_MODEL // MLSTM_HEADS
MLSTM_CHUNK = 64
MLSTM_M_INIT = -1e30
D_FF = 4 * D_MODEL

kernel_name = 'interleaved_fox_rglru_stickbreak_mlstm_trunk'


def _n_of(m):
    return (DEPTH - m + N_MIXERS - 1) // N_MIXERS


def rmsnorm(x, g):
    xf = x.astype(jnp.float32)
    y = xf * lax.rsqrt(jnp.mean(xf * xf, axis=-1, keepdims=True) + EPS)
    return (y * g.astype(jnp.float32)).astype(x.dtype)


def _heads(t, n_heads, d):
    b, s, _ = t.shape
    return t.reshape(b, s, n_heads, d).transpose(0, 2, 1, 3)


def _blocks_to_seq(out):
    nb, b, h, q, d = out.shape
    return out.transpose(1, 0, 3, 2, 4).reshape(b, nb * q, h * d)


def fox_attention(x, w_in, b_f, w_out):
    B, S, _ = x.shape
    H, dh = FOX_HEADS, FOX_HEAD_DIM
    proj = x @ w_in
    q, k, v, f_pre = jnp.split(proj, [D_MODEL, 2 * D_MODEL, 3 * D_MODEL], axis=-1)
    q, k, v = _heads(q, H, dh), _heads(k, H, dh), _heads(v, H, dh)
    log_f = jax.nn.log_sigmoid((f_pre + b_f).astype(jnp.float32)).transpose(0, 2, 1)
    cum = jnp.cumsum(log_f, axis=-1)
    nb = S // Q_BLOCK
    qb = q.reshape(B, H, nb, Q_BLOCK, dh).transpose(2, 0, 1, 3, 4)
    cb = cum.reshape(B, H, nb, Q_BLOCK).transpose(2, 0, 1, 3)
    pos_k = jnp.arange(S)
    scale = dh ** -0.5

    def block(args):
        i, q_i, c_i = args
        pos_q = i * Q_BLOCK + jnp.arange(Q_BLOCK)
        logits = jnp.einsum('bhqd,bhkd->bhqk', q_i, k).astype(jnp.float32) * scale
        logits = logits + c_i[..., :, None] - cum[..., None, :]
        logits = jnp.where(pos_k[None, :] <= pos_q[:, None], logits, -jnp.inf)
        p = jax.nn.softmax(logits, axis=-1)
        return jnp.einsum('bhqk,bhkd->bhqd', p.astype(v.dtype), v)

    out = lax.map(block, (jnp.arange(nb), qb, cb))
    return (_blocks_to_seq(out) @ w_out).astype(x.dtype)


def rglru_block(x, w_in, conv_w, conv_b, w_r, b_r, w_i, b_i, lam, w_out):
    B, S, _ = x.shape
    W, NB, bd = LRU_WIDTH, LRU_BLOCKS, LRU_BLOCK_DIM
    proj = x @ w_in
    gate, u = jnp.split(proj, [W], axis=-1)
    u_pad = jnp.pad(u, ((0, 0), (CONV_WIDTH - 1, 0), (0, 0)))
    u = conv_b + sum(u_pad[:, j:j + S, :] * conv_w[j] for j in range(CONV_WIDTH))
    ub = u.reshape(B, S, NB, bd)
    r = jax.nn.sigmoid((jnp.einsum('bsnd,nde->bsne', ub, w_r).reshape(B, S, W) + b_r).astype(jnp.float32))
    ig = jax.nn.sigmoid((jnp.einsum('bsnd,nde->bsne', ub, w_i).reshape(B, S, W) + b_i).astype(jnp.float32))
    log_a = LRU_C * r * jax.nn.log_sigmoid(lam.astype(jnp.float32))
    a = jnp.exp(log_a)
    bterm = jnp.sqrt(-jnp.expm1(2.0 * log_a)) * (ig * u.astype(jnp.float32))

    def combine(left, right):
        a1, b1 = left
        a2, b2 = right
        return a1 * a2, a2 * b1 + b2

    _, h = lax.associative_scan(combine, (a, bterm), axis=1)
    y = h * jax.nn.gelu(gate.astype(jnp.float32))
    return (y.astype(x.dtype) @ w_out).astype(x.dtype)


def stick_breaking_attention(x, w_in, w_out):
    B, S, _ = x.shape
    H, dh = SB_HEADS, SB_HEAD_DIM
    q, k, v = jnp.split(x @ w_in, 3, axis=-1)
    q, k, v = _heads(q, H, dh), _heads(k, H, dh), _heads(v, H, dh)
    nb = S // Q_BLOCK
    qb = q.reshape(B, H, nb, Q_BLOCK, dh).transpose(2, 0, 1, 3, 4)
    pos_k = jnp.arange(S)
    scale = dh ** -0.5

    def block(args):
        i, q_i = args
        pos_q = i * Q_BLOCK + jnp.arange(Q_BLOCK)
        strict = pos_k[None, :] < pos_q[:, None]
        z = jnp.einsum('bhqd,bhkd->bhqk', q_i, k).astype(jnp.float32) * scale
        log_beta = jax.nn.log_sigmoid(z)
        log_1mb = jnp.where(strict, jax.nn.log_sigmoid(-z), 0.0)
        after = lax.cumsum(log_1mb, axis=3, reverse=True) - log_1mb
        att = jnp.where(strict, jnp.exp(log_beta + after), 0.0)
        return jnp.einsum('bhqk,bhkd->bhqd', att.astype(v.dtype), v)

    out = lax.map(block, (jnp.arange(nb), qb))
    return (_blocks_to_seq(out) @ w_out).astype(x.dtype)


def mlstm_block(x, w_in, b_if, head_g, w_out):
    B, S, _ = x.shape
    H, dk, dv, L = MLSTM_HEADS, MLSTM_QK_DIM, MLSTM_V_DIM, MLSTM_CHUNK
    qk = H * dk
    proj = x @ w_in
    q, k, v, o_pre, g_pre = jnp.split(proj, [qk, 2 * qk, 2 * qk + D_MODEL, 2 * qk + 2 * D_MODEL], axis=-1)
    q, k, v = _heads(q, H, dk), _heads(k, H, dk) * (dk ** -0.5), _heads(v, H, dv)
    g_pre = g_pre.astype(jnp.float32).reshape(B, S, 2, H) + b_if.astype(jnp.float32)
    i_pre = g_pre[:, :, 0].transpose(0, 2, 1)
    log_f = jax.nn.log_sigmoid(g_pre[:, :, 1]).transpose(0, 2, 1)
    nc = S // L

    def chunks(t):
        return jnp.moveaxis(t.reshape(t.shape[:2] + (nc, L) + t.shape[3:]), 2, 0)

    causal = jnp.tril(jnp.ones((L, L), dtype=bool))

    def step(carry, xs):
        C, n, m = carry
        qc, kc, vc, lfc, ic = xs
        b = jnp.cumsum(lfc, axis=-1)
        g = b[..., -1]
        Dm = jnp.where(causal, b[..., :, None] - b[..., None, :] + ic[..., None, :], -jnp.inf)
        inter = b + m[..., None]
        m_t = jnp.maximum(inter, jnp.max(Dm, axis=-1))
        w_intra = jnp.exp(Dm - m_t[..., None])
        w_inter = jnp.exp(inter - m_t)
        s = jnp.einsum('bhtd,bhsd->bhts', qc, kc).astype(jnp.float32) * w_intra
        num = (w_inter[..., None] * jnp.einsum('bhtd,bhde->bhte', qc, C)
               + jnp.einsum('bhts,bhse->bhte', s, vc))
        den = w_inter * jnp.einsum('bhtd,bhd->bht', qc, n) + jnp.sum(s, axis=-1)
        h = num / jnp.maximum(jnp.abs(den), jnp.exp(-m_t))[..., None]
        wk = g[..., None] - b + ic
        m_new = jnp.maximum(g + m, jnp.max(wk, axis=-1))
        decay = jnp.exp(g + m - m_new)
        wk_e = jnp.exp(wk - m_new[..., None])
        C_new = decay[..., None, None] * C + jnp.einsum('bhs,bhsd,bhse->bhde', wk_e, kc, vc)
        n_new = decay[..., None] * n + jnp.einsum('bhs,bhsd->bhd', wk_e, kc)
        return (C_new, n_new, m_new), h

    init = (jnp.zeros((B, H, dk, dv), jnp.float32), jnp.zeros((B, H, dk), jnp.float32),
            jnp.full((B, H), MLSTM_M_INIT, jnp.float32))
    _, hs = lax.scan(step, init, (chunks(q), chunks(k), chunks(v), chunks(log_f), chunks(i_pre)))
    h = hs.transpose(1, 0, 3, 2, 4).reshape(B, S, H, dv)
    hn = rmsnorm(h, head_g.reshape(H, dv)).reshape(B, S, D_MODEL)
    y = jax.nn.sigmoid(o_pre.astype(jnp.float32)) * hn
    return (y.astype(x.dtype) @ w_out).astype(x.dtype)


def sqrelu_mlp(x, w1, w2):
    return (jnp.square(jax.nn.relu(x @ w1)) @ w2).astype(x.dtype)


def _dense(key, shape, fan_in):
    return jax.random.normal(key, shape, jnp.float32) * (fan_in ** -0.5)


def setup_inputs(seed: int = 0) -> dict:
    key = jax.random.key(seed)
    ks = jax.random.split(key, 24)
    n0, n1, n2, n3 = _n_of(0), _n_of(1), _n_of(2), _n_of(3)
    D, W, NB, bd = D_MODEL, LRU_WIDTH, LRU_BLOCKS, LRU_BLOCK_DIM
    x = jax.random.normal(ks[0], (BATCH, SEQ, D), jnp.float32)
    norm_g = 1.0 + 0.02 * jax.random.normal(ks[1], (DEPTH, 4, D), jnp.float32)
    mlp_w1 = _dense(ks[2], (DEPTH, D, D_FF), D)
    mlp_w2 = _dense(ks[3], (DEPTH, D_FF, D), D_FF)
    fox_w_in = _dense(ks[4], (n0, D, 3 * D + FOX_HEADS), D)
    fox_b_f = 3.0 + 0.1 * jax.random.normal(ks[5], (n0, FOX_HEADS), jnp.float32)
    fox_w_out = _dense(ks[6], (n0, D, D), D)
    lru_w_in = _dense(ks[7], (n1, D, 2 * W), D)
    lru_conv_w = _dense(ks[8], (n1, CONV_WIDTH, W), CONV_WIDTH)
    lru_conv_b = 0.01 * jax.random.normal(ks[9], (n1, W), jnp.float32)
    lru_w_r = _dense(ks[10], (n1, NB, bd, bd), bd)
    lru_b_r = 0.01 * jax.random.normal(ks[11], (n1, W), jnp.float32)
    lru_w_i = _dense(ks[12], (n1, NB, bd, bd), bd)
    lru_b_i = 0.01 * jax.random.normal(ks[13], (n1, W), jnp.float32)
    a_c = jax.random.uniform(ks[14], (n1, W), jnp.float32, minval=0.9, maxval=0.999)
    p = a_c ** (1.0 / LRU_C)
    lru_lambda = jnp.log(p) - jnp.log1p(-p)
    lru_w_out = _dense(ks[15], (n1, W, D), W)
    sb_w_in = _dense(ks[16], (n2, D, 3 * D), D)
    sb_w_out = _dense(ks[17], (n2, D, D), D)
    mlstm_w_in = _dense(ks[18], (n3, D, 3 * D + 2 * MLSTM_HEADS), D)
    mlstm_b_if = jnp.stack([-1.0 + 0.1 * jax.random.normal(ks[19], (n3, MLSTM_HEADS), jnp.float32),
                            3.0 + 0.1 * jax.random.normal(ks[20], (n3, MLSTM_HEADS), jnp.float32)], axis=1)
    mlstm_head_g = 1.0 + 0.02 * jax.random.normal(ks[21], (n3, D), jnp.float32)
    mlstm_w_out = _dense(ks[22], (n3, D, D), D)
    return {'x': x, 'norm_g': norm_g, 'mlp_w1': mlp_w1, 'mlp_w2': mlp_w2,
            'fox_w_in': fox_w_in, 'fox_b_f': fox_b_f, 'fox_w_out': fox_w_out,
            'lru_w_in': lru_w_in, 'lru_conv_w': lru_conv_w, 'lru_conv_b': lru_conv_b,
            'lru_w_r': lru_w_r, 'lru_b_r': lru_b_r, 'lru_w_i': lru_w_i, 'lru_b_i': lru_b_i,
            'lru_lambda': lru_lambda, 'lru_w_out': lru_w_out,
            'sb_w_in': sb_w_in, 'sb_w_out': sb_w_out,
            'mlstm_w_in': mlstm_w_in, 'mlstm_b_if': mlstm_b_if, 'mlstm_head_g': mlstm_head_g,
            'mlstm_w_out': mlstm_w_out}


def reference(x, norm_g, mlp_w1, mlp_w2, fox_w_in, fox_b_f, fox_w_out,
              lru_w_in, lru_conv_w, lru_conv_b, lru_w_r, lru_b_r, lru_w_i, lru_b_i,
              lru_lambda, lru_w_out, sb_w_in, sb_w_out,
              mlstm_w_in, mlstm_b_if, mlstm_head_g, mlstm_w_out):
    for i in range(DEPTH):
        m, j = i % N_MIXERS, i // N_MIXERS
        h = rmsnorm(x, norm_g[i, 0])
        if m == 0:
            h = fox_attention(h, fox_w_in[j], fox_b_f[j], fox_w_out[j])
        elif m == 1:
            h = rglru_block(h, lru_w_in[j], lru_conv_w[j], lru_conv_b[j], lru_w_r[j], lru_b_r[j],
                            lru_w_i[j], lru_b_i[j], lru_lambda[j], lru_w_out[j])
        elif m == 2:
            h = stick_breaking_attention(h, sb_w_in[j], sb_w_out[j])
        else:
            h = mlstm_block(h, mlstm_w_in[j], mlstm_b_if[j], mlstm_head_g[j], mlstm_w_out[j])
        x = x + rmsnorm(h, norm_g[i, 1])
        h = sqrelu_mlp(rmsnorm(x, norm_g[i, 2]), mlp_w1[i], mlp_w2[i])
        x = x + rmsnorm(h, norm_g[i, 3])
    return x
```

```python
import contextlib
import numpy as np
import ml_dtypes
import concourse.bass as bass
import concourse.mybir as mybir
from concourse.bass_utils import run_bass_kernel_spmd

F32 = mybir.dt.float32
BF16 = mybir.dt.bfloat16
AF = mybir.ActivationFunctionType
ALU = mybir.AluOpType
AX = mybir.AxisListType

D = 2048
KC = 16
NT = 2048
T = 512
NSL = 4
NCORE = 8
S = 8192
GT = 16
EPS = 1e-6
DFF = 8192
LW = 2688
LP = 84
LC = 32
ENGS = ('pe', 'dve', 'act', 'pool', 'sp')
NRING = {'sp': 12, 'pool': 12, 'act': 4}


class Tk:
    __slots__ = ('w', 'r', 'ap', 'name')

    def __init__(self, ap=None, name=None):
        self.w = None
        self.r = {}
        self.ap = ap
        self.name = name

    def __getitem__(self, idx):
        return self.ap[idx]


class Prog:
    def __init__(self, nc, stack):
        self.nc = nc
        self.stack = stack
        self.ops = {e: [] for e in ENGS}
        self.cnt = {e: 0 for e in ENGS}
        self.seen = {e: {} for e in ENGS}
        self.sems = {}
        for e in ENGS:
            self.sems[('E', e)] = stack.enter_context(nc.semaphore('s_' + e))
        self.ring = {}
        self.ring_cnt = {}
        self.ring_i = {}
        for q, n in NRING.items():
            self.ring[q] = []
            for i in range(n):
                key = ('D', q, i)
                self.sems[key] = stack.enter_context(nc.semaphore('d_%s%d' % (q, i)))
                self.ring[q].append(key)
            self.ring_cnt[q] = [0] * n
            self.ring_i[q] = 0
        self.n_tiles = 0

    def sbuf(self, shape, dt, name=None):
        self.n_tiles += 1
        name = name or 't%d' % self.n_tiles
        t = self.stack.enter_context(self.nc.sbuf_tensor(name, list(shape), dt))
        return Tk(t, name)

    def psum(self, shape, dt, name=None):
        self.n_tiles += 1
        name = name or 'p%d' % self.n_tiles
        t = self.stack.enter_context(self.nc.psum_tensor(name, list(shape), dt))
        return Tk(t, name)

    def dram(self, name, shape, dt, kind="Internal"):
        return Tk(self.nc.dram_tensor(name, list(shape), dt, kind=kind).ap(), name)

    def _waits(self, eng, reads, writes, self_sync):
        waits = {}
        seen = self.seen[eng]
        own = ('E', eng)

        def need(key, v):
            if (not self_sync) and key == own:
                return
            if seen.get(key, 0) >= v:
                return
            if waits.get(key, 0) < v:
                waits[key] = v

        for t in reads:
            if t.w is not None:
                need(*t.w)
        for t in writes:
            if t.w is not None:
                need(*t.w)
            for k, v in t.r.items():
                need(k, v)
        for k, v in waits.items():
            seen[k] = v
        return list(waits.items())

    def op(self, eng, fn, reads=(), writes=(), self_sync=True):
        waits = self._waits(eng, reads, writes, self_sync)
        self.cnt[eng] += 1
        n = self.cnt[eng]
        key = ('E', eng)
        self.ops[eng].append((waits, fn, key, 1))
        for t in reads:
            if t.r.get(key, 0) < n:
                t.r[key] = n
        for t in writes:
            t.w = (key, n)
            t.r = {}

    def dma(self, q, fn, reads=(), writes=()):
        i = self.ring_i[q]
        self.ring_i[q] = (i + 1) % len(self.ring[q])
        key = self.ring[q][i]
        waits = self._waits(q, reads, writes, True)
        prev = 16 * self.ring_cnt[q][i]
        if prev > 0 and self.seen[q].get(key, 0) < prev:
            waits.append((key, prev))
            self.seen[q][key] = prev
        self.ring_cnt[q][i] += 1
        v = 16 * self.ring_cnt[q][i]
        self.ops[q].append((waits, fn, key, 16))
        for t in reads:
            if t.r.get(key, 0) < v:
                t.r[key] = v
        for t in writes:
            t.w = (key, v)
            t.r = {}

    def finish(self, outs):
        waits = self._waits('sp', outs, (), True)
        self.ops['sp'].append((waits, None, None, 0))

    def emit(self):
        nc = self.nc
        sems = self.sems
        ops = self.ops

        def run(eng_name, eng):
            for waits, fn, key, inc in ops[eng_name]:
                for k, v in waits:
                    eng.wait_ge(sems[k], v)
                if fn is not None:
                    fn(eng).then_inc(sems[key], inc)

        with nc.Block() as block:
            @block.tensor
            def _(e):
                run('pe', e)

            @block.vector
            def _(e):
                run('dve', e)

            @block.scalar
            def _(e):
                run('act', e)

            @block.gpsimd
            def _(e):
                run('pool', e)

            @block.sync
            def _(e):
                run('sp', e)


class Ker:
    def __init__(self, nc, st):
        self.nc = nc
        P = self.P = Prog(nc, st)
        self.A32 = P.sbuf([128, KC, T], F32, 'A32')
        self.B32 = P.sbuf([128, KC, T], F32, 'B32')
        self.H16 = P.sbuf([128, KC, T], BF16, 'H16')
        self.U = [P.sbuf([128, 16, T], BF16, 'U%d' % i) for i in range(4)]
        self.W = [P.sbuf([128, 8192], BF16, 'W%d' % i) for i in range(2)]
        self.wi = 0
        self.sq = [P.sbuf([128, T], F32, 'sq%d' % i) for i in range(2)]
        self.sqi = 0
        self.rs = P.sbuf([128, T], F32, 'rs')
        self.pT = [P.sbuf([128, T], BF16, 'pT%d' % i) for i in range(2)]
        self.pti = 0
        self.f = [P.sbuf([128, T], F32, 'f%d' % i) for i in range(2)]
        self.lnv = self.f[1]
        self.ps = [P.psum([128, T], F32, 'ps%d' % i) for i in range(8)]
        self.psi = 0
        self.evi = 0
        self.ones_f = P.sbuf([128, 128], F32, 'ones_f')
        self.ones_b = P.sbuf([128, 128], BF16, 'ones_b')
        self.tri_f = P.sbuf([128, 128], F32, 'tri_f')
        self.triu_f = P.sbuf([128, 128], F32, 'triu_f')
        self.eps_t = P.sbuf([128, 1], F32, 'eps_t')
        self.one_t = P.sbuf([128, 1], F32, 'one_t')
        P.op('pool', lambda e: e.memset(self.ones_f[:], 1.0), writes=[self.ones_f])
        P.op('pool', lambda e: e.memset(self.ones_b[:], 1.0), writes=[self.ones_b])
        P.op('pool', lambda e: e.memset(self.eps_t[:], EPS), writes=[self.eps_t])
        P.op('pool', lambda e: e.memset(self.one_t[:], 1.0), writes=[self.one_t])
        P.op('pool', lambda e: e.memset(self.tri_f[:], 1.0), writes=[self.tri_f])
        P.op('pool', lambda e: e.affine_select(out=self.tri_f[:], in_=self.tri_f[:], pattern=[[1, 128]],
                                               compare_op=ALU.is_ge, fill=0.0, base=0, channel_multiplier=-1),
             reads=[self.tri_f], writes=[self.tri_f])
        P.op('pool', lambda e: e.memset(self.triu_f[:], 1.0), writes=[self.triu_f])
        P.op('pool', lambda e: e.affine_select(out=self.triu_f[:], in_=self.triu_f[:], pattern=[[-1, 128]],
                                               compare_op=ALU.is_ge, fill=0.0, base=0, channel_multiplier=1),
             reads=[self.triu_f], writes=[self.triu_f])

    def wbuf(self):
        w = self.W[self.wi]
        self.wi = (self.wi + 1) % 2
        return w

    def psA(self):
        p = self.ps[self.psi]
        self.psi = (self.psi + 1) % 2
        return p

    def ptile(self):
        p = self.pT[self.pti]
        self.pti = (self.pti + 1) % 2
        return p

    def load(self, dst, dst_ap, src_tk, src_ap, q='sp'):
        self.P.dma(q, lambda e: e.dma_start(out=dst_ap, in_=src_ap), reads=[src_tk], writes=[dst])

    def store(self, dst_tk, dst_ap, src, src_ap, q='sp'):
        self.P.dma(q, lambda e: e.dma_start(out=dst_ap, in_=src_ap), reads=[src], writes=[dst_tk])

    def evac_copy(self, dst, dst_ap, ps, ps_ap=None, scale=None):
        P = self.P
        ps_ap = ps[:] if ps_ap is None else ps_ap
        self.evi += 1
        if scale is not None:
            P.op('act', lambda e: e.mul(out=dst_ap, in_=ps_ap, mul=scale), reads=[ps], writes=[dst])
        elif self.evi % 2:
            P.op('act', lambda e: e.activation(out=dst_ap, in_=ps_ap, func=AF.Copy), reads=[ps], writes=[dst])
        else:
            P.op('dve', lambda e: e.tensor_copy(out=dst_ap, in_=ps_ap), reads=[ps], writes=[dst])

    def rstd(self, src, nch, dim, ps=None):
        P = self.P
        ps = ps or self.ps[7]
        for k in range(nch):
            sq = self.sq[self.sqi]
            self.sqi ^= 1
            P.op('act', lambda e, k=k, sq=sq: e.activation(out=sq[:], in_=src[:, k, :], func=AF.Square), reads=[src], writes=[sq])
            P.op('pe', lambda e, k=k, sq=sq: e.matmul(ps[:], lhsT=self.ones_f[:], rhs=sq[:], start=(k == 0), stop=(k == nch - 1)),
                 reads=[sq, self.ones_f], writes=[ps], self_sync=False)
        P.op('act', lambda e: e.activation(out=self.lnv[:], in_=ps[:], func=AF.Ln, scale=1.0 / dim, bias=self.eps_t[:, 0:1]),
             reads=[ps, self.eps_t], writes=[self.lnv])
        P.op('act', lambda e: e.activation(out=self.rs[:], in_=self.lnv[:], func=AF.Exp, scale=-0.5), reads=[self.lnv], writes=[self.rs])
        return self.rs

    def norm_apply(self, dst, src, g, nch, rs, dst_off=0, g_off=0):
        P = self.P
        for k in range(nch):
            P.op('dve', lambda e, k=k: e.scalar_tensor_tensor(out=dst[:, dst_off + k, :], in0=src[:, k, :],
                                                              scalar=g[:, g_off + k:g_off + k + 1], in1=rs[:],
                                                              op0=ALU.mult, op1=ALU.mult),
                 reads=[src, g, rs], writes=[dst])

    def linear_fm(self, rhs_of, nk, w_tk, w_view, col0, ncols, evac, kp=128, mw=128, rhs_tks=()):
        P = self.P
        gw = (min(512, 8192 // nk) // mw) * mw
        m_idx = 0
        c = 0
        while c < ncols:
            w = min(gw, ncols - c)
            wb = self.wbuf()
            wv = wb.ap[0:kp, 0:nk * w].rearrange("p (k m) -> p k m", m=w)
            P.dma('pool', lambda e, wv=wv, c=c, w=w: e.dma_start(out=wv, in_=w_view[:, :, col0 + c:col0 + c + w]),
                  reads=[w_tk], writes=[wb])
            for j in range(w // mw):
                ps = self.psA()
                for k in range(nk):
                    P.op('pe', lambda e, wv=wv, j=j, k=k, ps=ps: e.matmul(ps[0:mw, :], lhsT=wv[:, k, j * mw:(j + 1) * mw], rhs=rhs_of(k),
                                                                         start=(k == 0), stop=(k == nk - 1)),
                         reads=[wb] + list(rhs_tks), writes=[ps], self_sync=False)
                evac(m_idx, ps)
                m_idx += 1
            c += w


def xview(tk):
    return tk.ap.rearrange("k p t -> p k t")


def wview(wb, nk, w, kp=128):
    return wb.ap[0:kp, 0:nk * w].rearrange("p (k m) -> p k m", m=w)


def residual_norm_add(K, x, y, g, g_off):
    P = K.P
    rs = K.rstd(y, KC, D)
    K.norm_apply(y, y, g, KC, rs, g_off=g_off)
    for k in range(KC):
        P.op('pool', lambda e, k=k: e.tensor_tensor(out=x[:, k, :], in0=x[:, k, :], in1=y[:, k, :], op=ALU.add), reads=[x, y], writes=[x])


def mlp_tile(K, xm, gt, goff, w1, w2, out_tk, out_ap):
    P = K.P
    rs = K.rstd(xm, KC, D)
    K.norm_apply(K.H16, xm, gt, KC, rs, g_off=goff)
    w1v = w1.ap.rearrange("(k p) m -> p k m", p=128)

    def ev1(m, ps):
        u = K.U[m // 16]
        f = K.f[0]
        P.op('act', lambda e: e.activation(out=f[:], in_=ps[:], func=AF.Relu), reads=[ps], writes=[f])
        eng = 'pool' if m % 2 else 'dve'
        P.op(eng, lambda e: e.tensor_tensor(out=u[:, m % 16, :], in0=f[:], in1=f[:], op=ALU.mult), reads=[f], writes=[u])

    K.linear_fm(lambda k: K.H16[:, k, :], KC, w1, w1v, 0, DFF, ev1, rhs_tks=[K.H16])
    w2v = w2.ap.rearrange("(q k p) m -> q p k m", q=4, p=128)
    for cg in range(4):
        acc = K.ps[2:6]
        for kq in range(4):
            wb = K.wbuf()
            wv = wview(wb, 16, 512)
            P.dma('pool', lambda e, wv=wv, kq=kq, cg=cg: e.dma_start(out=wv, in_=w2v[kq][:, :, cg * 512:(cg + 1) * 512]),
                  reads=[w2], writes=[wb])
            for j in range(4):
                for k in range(16):
                    P.op('pe', lambda e, wv=wv, j=j, k=k, kq=kq: e.matmul(acc[j][:], lhsT=wv[:, k, j * 128:(j + 1) * 128], rhs=K.U[kq][:, k, :],
                                                                         start=(kq == 0 and k == 0), stop=(kq == 3 and k == 15)),
                         reads=[wb, K.U[kq]], writes=[acc[j]], self_sync=False)
        for j in range(4):
            K.evac_copy(K.B32, K.B32[:, cg * 4 + j, :], acc[j])
    residual_norm_add(K, xm, K.B32, gt, goff + KC)
    K.store(out_tk, out_ap, xm, xm[:])


def proj_tm(K, w_tk, w_view, col0, ncols, emit):
    P = K.P
    for cg in range(ncols // 512):
        wb = K.wbuf()
        wv = wview(wb, 16, 512)
        P.dma('pool', lambda e, wv=wv, cg=cg: e.dma_start(out=wv, in_=w_view[:, :, col0 + cg * 512:col0 + (cg + 1) * 512]),
              reads=[w_tk], writes=[wb])
        for tb in range(4):
            ps = K.psA()
            for k in range(16):
                P.op('pe', lambda e, wv=wv, tb=tb, k=k, ps=ps: e.matmul(ps[:], lhsT=K.H16[:, k, tb * 128:(tb + 1) * 128], rhs=wv[:, k, :],
                                                                       start=(k == 0), stop=(k == 15)),
                     reads=[wb, K.H16], writes=[ps], self_sync=False)
            emit(cg, tb, ps)


def small_tm(K, wsm, ncol, tb, ps):
    P = K.P
    for k in range(16):
        P.op('pe', lambda e, k=k: e.matmul(ps[:, 0:ncol], lhsT=K.H16[:, k, tb * 128:(tb + 1) * 128], rhs=wsm[:, k, 0:ncol],
                                           start=(k == 0), stop=(k == 15)),
             reads=[wsm, K.H16], writes=[ps], self_sync=False)


def softplus_neg(K, dst, dst_ap, src, src_ap, bias_ap, bias_tk, tmp, tmp_ap):
    P = K.P
    P.op('dve', lambda e: e.tensor_tensor(out=tmp_ap, in0=src_ap, in1=bias_ap, op=ALU.add), reads=[src, bias_tk], writes=[tmp])
    P.op('act', lambda e: e.activation(out=tmp_ap, in_=tmp_ap, func=AF.Exp, scale=-1.0), reads=[tmp], writes=[tmp])
    P.op('act', lambda e: e.activation(out=dst_ap, in_=tmp_ap, func=AF.Ln, bias=K.one_t[:, 0:1]), reads=[tmp, K.one_t], writes=[dst])


def cumsum_block(K, dst, dst_ap, sp, sp_ap, run, ncol, ps):
    P = K.P
    P.op('pe', lambda e: e.matmul(ps[:, 0:ncol], lhsT=K.tri_f[:], rhs=sp_ap, start=True, stop=True), reads=[K.tri_f, sp], writes=[ps], self_sync=False)
    P.op('dve', lambda e: e.tensor_tensor(out=dst_ap, in0=ps[:, 0:ncol], in1=run[:, 0:ncol], op=ALU.add), reads=[ps, run], writes=[dst])
    P.op('pe', lambda e: e.matmul(ps[:, 64:64 + ncol], lhsT=K.ones_f[:], rhs=sp_ap, start=True, stop=True), reads=[K.ones_f, sp], writes=[ps], self_sync=False)
    P.op('dve', lambda e: e.tensor_tensor(out=run[:, 0:ncol], in0=run[:, 0:ncol], in1=ps[:, 64:64 + ncol], op=ALU.add), reads=[ps, run], writes=[run])


class Ctx:
    pass


def alloc_small(K):
    P = K.P
    K.negc = P.sbuf([128, 64, 16], F32, 'negc')
    K.run = P.sbuf([128, 16], F32, 'run')
    K.bt = P.sbuf([128, 64], F32, 'bt')
    K.cm = P.sbuf([128, 4, 513], BF16, 'cm')
    K.sm = P.sbuf([128, 64], F32, 'sm')
    K.wsm = P.sbuf([128, 16, 16], BF16, 'wsm')
    K.bb = P.sbuf([128, 16], F32, 'bb')
    P.op('pool', lambda e: e.memset(K.cm[:], 1.0), writes=[K.cm])
    for j in range(4):
        P.op('pool', lambda e, j=j: e.affine_select(out=K.cm[:, j, :], in_=K.cm[:, j, :], pattern=[[1, 513]], compare_op=ALU.is_ge, fill=0.0,
                                                    base=-1 - 128 * j, channel_multiplier=-1), reads=[K.cm], writes=[K.cm])


def load_x_norm(K, C, li, i):
    src = C.xin if li == C.layers[0] else C.xout
    K.load(K.A32, K.A32[:], src, xview(src)[:, :, i * T:(i + 1) * T])
    rs = K.rstd(K.A32, KC, D)
    K.norm_apply(K.H16, K.A32, C.gt, KC, rs, g_off=li * 64)


def finish_tile(K, C, li, i):
    if globals().get('DEBUG_MIX'):
        K.store(C.xout, xview(C.xout)[:, :, i * T:(i + 1) * T], K.B32, K.B32[:])
        return
    residual_norm_add(K, K.A32, K.B32, C.gt, li * 64 + 16)
    mlp_tile(K, K.A32, C.gt, li * 64 + 32, C.w1[li], C.w2[li], C.xout, xview(C.xout)[:, :, i * T:(i + 1) * T])


def kv_proj(K, C, w, wv_, i, qcol, kcol, vcol, nq, qscale, kscale, vh, vd):
    P = K.P

    def evq(m, ps):
        K.evac_copy(K.U[3], K.U[3][:, m, :], ps, scale=qscale)
    K.linear_fm(lambda k: K.H16[:, k, :], KC, w, wv_, qcol, nq * 128, evq, rhs_tks=[K.H16])

    def evk(m, ps):
        st_ = K.ptile()
        K.evac_copy(st_, st_[:], ps, scale=kscale)
        K.store(C.kT, C.kT.ap[m, :, i * T:(i + 1) * T], st_, st_[:])
    K.linear_fm(lambda k: K.H16[:, k, :], KC, w, wv_, kcol, nq * 128, evk, rhs_tks=[K.H16])

    hpg = 512 // vd

    def evv(cg, tb, ps):
        st_ = K.ptile()
        K.evac_copy(st_, st_[:], ps)
        if hpg >= 1:
            K.store(C.vx, C.vx.ap[cg * hpg:(cg + 1) * hpg, :, i * 4 + tb, :].rearrange("h p d -> p h d"), st_, st_[:].rearrange("p (h d) -> p h d", d=vd))
    if vd <= 512:
        proj_tm(K, w, wv_, vcol, 2048, evv)


def layer_attn(K, C, li, kind, NTILE):
    P = K.P
    w = C.w_in[li]
    wv_ = w.ap.rearrange("(k p) m -> p k m", p=128)
    wo = C.w_out[li]
    wov = wo.ap.rearrange("(k p) m -> p k m", p=128)
    scale = 128 ** -0.5
    if kind == 'fox':
        K.load(K.wsm, K.wsm[:], w, wv_[:, :, 6144:6160], q='pool')
        K.load(K.bb, K.bb[:], C.fox_b, C.fox_b.ap.partition_broadcast(128))
        P.op('pool', lambda e: e.memset(K.run[:], 0.0), writes=[K.run])
    KTH, VH, OT = K.U[0], K.U[1], K.U[2]
    for i in range(NTILE):
        load_x_norm(K, C, li, i)
        kv_proj(K, C, w, wv_, i, 0, 2048, 4096, 16, scale, None, 16, 128)
        nb = 4 * (i + 1)
        if kind == 'fox':
            for tb in range(4):
                ps = K.ps[6]
                small_tm(K, K.wsm, 16, tb, ps)
                softplus_neg(K, K.sm, K.sm[:, 0:16], ps, ps[:, 0:16], K.bb[:], K.bb, K.sm, K.sm[:, 16:32])
                cumsum_block(K, K.negc, K.negc[:, i * 4 + tb, :], K.sm, K.sm[:, 0:16], K.run, 16, K.ps[7])
        for h in range(16):
            K.load(KTH, KTH.ap.rearrange("p a b -> p (a b)")[:, 0:nb * 128], C.kT, C.kT.ap[h, :, 0:nb * 128])
            K.load(VH, VH.ap.rearrange("p a b -> p (a b)")[:, 0:nb * 128].rearrange("p (a b) -> p a b", b=128), C.vx, C.vx.ap[h, :, 0:nb, :])
            kth = KTH.ap.rearrange("p a b -> p (a b)")
            vh = VH.ap.rearrange("p a b -> p (a b)")
            psO, psD = K.ps[2], K.ps[3]
            if kind == 'fox':
                P.op('dve', lambda e, h=h, nb=nb: e.tensor_scalar(out=K.bt[:, 0:nb], in0=K.negc[:, 0:nb, h], scalar1=K.run[:, h:h + 1], scalar2=0.0,
                                                                 op0=ALU.subtract, op1=ALU.min), reads=[K.negc, K.run], writes=[K.bt])
                for kb in range(nb):
                    ps = K.psA()
                    P.op('pe', lambda e, kb=kb, h=h, ps=ps: e.matmul(ps[:], lhsT=kth[:, kb * 128:(kb + 1) * 128], rhs=K.U[3][:, h, :], start=True, stop=True),
                         reads=[KTH, K.U[3]], writes=[ps], self_sync=False)
                    pt = K.ptile()
                    P.op('act', lambda e, kb=kb, ps=ps, pt=pt: e.activation(out=pt[:], in_=ps[:], func=AF.Exp, bias=K.bt[:, kb:kb + 1]),
                         reads=[ps, K.bt], writes=[pt])
                    if kb >= 4 * i:
                        j = kb - 4 * i
                        P.op('pool', lambda e, j=j, pt=pt: e.tensor_tensor(out=pt[:], in0=pt[:], in1=K.cm[:, j, 1:513], op=ALU.mult), reads=[pt, K.cm], writes=[pt])
                    P.op('pe', lambda e, kb=kb, pt=pt, nb=nb: e.matmul(psO[:], lhsT=vh[:, kb * 128:(kb + 1) * 128], rhs=pt[:], start=(kb == 0), stop=(kb == nb - 1)),
                         reads=[VH, pt], writes=[psO], self_sync=False)
                    P.op('pe', lambda e, kb=kb, pt=pt, nb=nb: e.matmul(psD[:], lhsT=K.ones_b[:], rhs=pt[:], start=(kb == 0), stop=(kb == nb - 1)),
                         reads=[K.ones_b, pt], writes=[psD], self_sync=False)
                rd = K.f[0]
                P.op('dve', lambda e: e.reciprocal(out=rd[:], in_=psD[:]), reads=[psD], writes=[rd])
                P.op('dve', lambda e, h=h: e.tensor_tensor(out=OT[:, h, :], in0=psO[:], in1=rd[:], op=ALU.mult), reads=[psO, rd], writes=[OT])
            else:
                spsum = K.sq[0]
                e1, sp, ex2 = K.f[0], K.f[1], K.sq[1]
                P.op('pool', lambda e: e.memset(spsum[:], 0.0), writes=[spsum])
                for n_, kb in enumerate(range(nb - 1, -1, -1)):
                    ps = K.psA()
                    P.op('pe', lambda e, kb=kb, h=h, ps=ps: e.matmul(ps[:], lhsT=kth[:, kb * 128:(kb + 1) * 128], rhs=K.U[3][:, h, :], start=True, stop=True),
                         reads=[KTH, K.U[3]], writes=[ps], self_sync=False)
                    P.op('act', lambda e, ps=ps: e.activation(out=e1[:], in_=ps[:], func=AF.Exp), reads=[ps], writes=[e1])
                    P.op('act', lambda e: e.activation(out=sp[:], in_=e1[:], func=AF.Ln, bias=K.one_t[:, 0:1]), reads=[e1, K.one_t], writes=[sp])
                    if kb >= 4 * i:
                        j = kb - 4 * i
                        P.op('pool', lambda e, j=j: e.tensor_tensor(out=sp[:], in0=sp[:], in1=K.cm[:, j, 0:512], op=ALU.mult), reads=[sp, K.cm], writes=[sp])
                    psE = K.ps[6 + n_ % 2]
                    P.op('pe', lambda e, psE=psE: e.matmul(psE[:], lhsT=K.triu_f[:], rhs=sp[:], start=True, stop=False), reads=[K.triu_f, sp], writes=[psE], self_sync=False)
                    P.op('pe', lambda e, psE=psE: e.matmul(psE[:], lhsT=K.ones_f[:], rhs=spsum[:], start=False, stop=True), reads=[K.ones_f, spsum], writes=[psE], self_sync=False)
                    P.op('act', lambda e, psE=psE: e.activation(out=ex2[:], in_=psE[:], func=AF.Exp, scale=-1.0), reads=[psE], writes=[ex2])
                    pt = K.ptile()
                    P.op('dve', lambda e, pt=pt: e.tensor_tensor(out=pt[:], in0=e1[:], in1=ex2[:], op=ALU.mult), reads=[e1, ex2], writes=[pt])
                    if kb >= 4 * i:
                        j = kb - 4 * i
                        P.op('pool', lambda e, j=j, pt=pt: e.tensor_tensor(out=pt[:], in0=pt[:], in1=K.cm[:, j, 0:512], op=ALU.mult), reads=[pt, K.cm], writes=[pt])
                    P.op('pool', lambda e: e.tensor_tensor(out=spsum[:], in0=spsum[:], in1=sp[:], op=ALU.add), reads=[spsum, sp], writes=[spsum])
                    P.op('pe', lambda e, kb=kb, pt=pt, n_=n_, nb=nb: e.matmul(psO[:], lhsT=vh[:, kb * 128:(kb + 1) * 128], rhs=pt[:], start=(n_ == 0), stop=(n_ == nb - 1)),
                         reads=[VH, pt], writes=[psO], self_sync=False)
                K.evac_copy(OT, OT[:, h, :], psO)

        def evo(m, ps):
            K.evac_copy(K.B32, K.B32[:, m, :], ps)
        K.linear_fm(lambda k: OT[:, k, :], KC, wo, wov, 0, 2048, evo, rhs_tks=[OT])
        finish_tile(K, C, li, i)


def layer_mlstm(K, C, li, NTILE):
    P = K.P
    w = C.w_in[li]
    wv_ = w.ap.rearrange("(k p) m -> p k m", p=128)
    wo = C.w_out[li]
    wov = wo.ap.rearrange("(k p) m -> p k m", p=128)
    K.load(K.wsm, K.wsm[:, :, 0:8], w, wv_[:, :, 6144:6152], q='pool')
    K.load(K.bb, K.bb[:, 0:8], C.ml_b, C.ml_b.ap.partition_broadcast(128))
    P.op('pool', lambda e: e.memset(K.run[:], 0.0), writes=[K.run])
    nF = Tk(K.negc.ap.rearrange("p a b -> p (a b)")[:, 0:256].rearrange("p (a b) -> p a b", b=4))
    ik = Tk(K.negc.ap.rearrange("p a b -> p (a b)")[:, 256:512].rearrange("p (a b) -> p a b", b=4))
    nFq = Tk(K.negc.ap.rearrange("p a b -> p (a b)")[0:8, 512:1024])
    carry = Tk(K.sm.ap[0:8, 32:33])
    bcol = Tk(K.sm.ap[0:8, 33:34])
    onesr = Tk(K.rs.ap[0:8, :])
    selh = P.sbuf([8, 4, 128], F32, 'selh')
    NEG = K.negc
    P.op('pool', lambda e: e.memset(carry.ap, 0.0), writes=[K.sm])
    P.op('pool', lambda e: e.memset(selh[:], 1.0), writes=[selh])
    for h in range(4):
        P.op('pool', lambda e, h=h: e.affine_select(out=selh[:, h, :], in_=selh[:, h, :], pattern=[[0, 128]], compare_op=ALU.is_equal, fill=0.0,
                                                    base=-(4 + h), channel_multiplier=1), reads=[selh], writes=[selh])
    K.load(K.sm, bcol.ap, C.ml_b, C.ml_b.ap.rearrange("(a b) -> a b", b=1))
    K0, K1, SO = K.U[0], K.U[1], K.U[2]
    k0 = K0.ap.rearrange("p a b -> p (a b)")
    k1 = K1.ap.rearrange("p a b -> p (a b)")
    for i in range(NTILE):
        load_x_norm(K, C, li, i)
        kv_proj(K, C, w, wv_, i, 0, 1024, 2048, 8, None, 256 ** -0.5, 4, 512)

        def evs(m, ps):
            P.op('act', lambda e: e.activation(out=SO[:, m, :], in_=ps[:], func=AF.Sigmoid), reads=[ps], writes=[SO])
        K.linear_fm(lambda k: K.H16[:, k, :], KC, w, wv_, 4096, 2048, evs, rhs_tks=[K.H16])
        nb = 4 * (i + 1)
        for tb in range(4):
            ps = K.ps[6]
            small_tm(K, K.wsm, 8, tb, ps)
            P.op('dve', lambda e, tb=tb, i=i, ps=ps: e.tensor_tensor(out=ik[:, i * 4 + tb, :], in0=ps[:, 0:4], in1=K.bb[:, 0:4], op=ALU.add), reads=[ps, K.bb], writes=[NEG])
            softplus_neg(K, K.sm, K.sm[:, 0:4], ps, ps[:, 4:8], K.bb[:, 4:8], K.bb, K.sm, K.sm[:, 16:20])
            cumsum_block(K, NEG, nF[:, i * 4 + tb, :], K.sm, K.sm[:, 0:4], K.run, 4, K.ps[7])
        psg = K.ps[6]
        for k in range(16):
            P.op('pe', lambda e, k=k: e.matmul(psg[0:8, :], lhsT=K.wsm[:, k, 0:8], rhs=K.H16[:, k, :], start=(k == 0), stop=(k == 15)),
                 reads=[K.wsm, K.H16], writes=[psg], self_sync=False)
        gq = Tk(K.sq[0].ap[0:8, :])
        P.op('dve', lambda e: e.tensor_scalar_add(out=gq.ap, in0=psg[0:8, :], scalar1=bcol.ap), reads=[psg, K.sm], writes=[K.sq[0]])
        P.op('pool', lambda e: e.memset(onesr.ap, 1.0), writes=[K.rs])
        P.op('act', lambda e: e.activation(out=gq.ap, in_=gq.ap, func=AF.Exp, scale=-1.0), reads=[K.sq[0]], writes=[K.sq[0]])
        P.op('act', lambda e: e.activation(out=gq.ap, in_=gq.ap, func=AF.Ln, bias=K.one_t[0:8, 0:1]), reads=[K.sq[0], K.one_t], writes=[K.sq[0]])
        P.op('dve', lambda e: e.tensor_tensor_scan(out=nFq.ap, data0=onesr.ap, data1=gq.ap, initial=carry.ap, op0=ALU.mult, op1=ALU.add),
             reads=[K.sq[0], K.rs, K.sm], writes=[NEG])
        P.op('dve', lambda e: e.tensor_copy(out=carry.ap, in_=nFq.ap[:, 511:512]), reads=[NEG], writes=[K.sm])
        for h in range(4):
            K.load(K0, k0[:, 0:nb * 128], C.kT, C.kT.ap[2 * h, :, 0:nb * 128])
            K.load(K1, k1[:, 0:nb * 128], C.kT, C.kT.ap[2 * h + 1, :, 0:nb * 128])
            P.op('dve', lambda e, h=h, nb=nb: e.tensor_scalar(out=K.bt[:, 0:nb], in0=nF[:, 0:nb, h], scalar1=K.run[:, h:h + 1], scalar2=0.0,
                                                             op0=ALU.subtract, op1=ALU.min), reads=[NEG, K.run], writes=[K.bt])
            P.op('dve', lambda e, h=h, nb=nb: e.tensor_tensor(out=K.bt[:, 0:nb], in0=K.bt[:, 0:nb], in1=ik[:, 0:nb, h], op=ALU.add), reads=[NEG, K.bt], writes=[K.bt])
            P.op('act', lambda e, nb=nb: e.activation(out=K.bt[:, 0:nb], in_=K.bt[:, 0:nb], func=AF.Exp), reads=[K.bt], writes=[K.bt])
            psO = K.ps[2:6]
            psD = K.ps[6]
            for kt in range(i + 1):
                vb = K.wbuf()
                vv = vb.ap[:, 0:2048].rearrange("p (a b) -> p a b", b=512)
                K.load(vb, vv, C.vx, C.vx.ap[h, :, kt * 4:(kt + 1) * 4, :])
                for j in range(4):
                    kb = kt * 4 + j
                    ps = K.psA()
                    P.op('pe', lambda e, kb=kb, h=h, ps=ps: e.matmul(ps[:], lhsT=k0[:, kb * 128:(kb + 1) * 128], rhs=K.U[3][:, 2 * h, :], start=True, stop=False),
                         reads=[K0, K.U[3]], writes=[ps], self_sync=False)
                    P.op('pe', lambda e, kb=kb, h=h, ps=ps: e.matmul(ps[:], lhsT=k1[:, kb * 128:(kb + 1) * 128], rhs=K.U[3][:, 2 * h + 1, :], start=False, stop=True),
                         reads=[K1, K.U[3]], writes=[ps], self_sync=False)
                    pt = K.ptile()
                    P.op('dve', lambda e, kb=kb, ps=ps, pt=pt: e.tensor_scalar_mul(out=pt[:], in0=ps[:], scalar1=K.bt[:, kb:kb + 1]), reads=[ps, K.bt], writes=[pt])
                    if kt == i:
                        P.op('pool', lambda e, j=j, pt=pt: e.tensor_tensor(out=pt[:], in0=pt[:], in1=K.cm[:, j, 1:513], op=ALU.mult), reads=[pt, K.cm], writes=[pt])
                    for c4 in range(4):
                        P.op('pe', lambda e, j=j, c4=c4, pt=pt, kb=kb, vv=vv, nb=nb: e.matmul(psO[c4][:], lhsT=vv[:, j, c4 * 128:(c4 + 1) * 128], rhs=pt[:], start=(kb == 0), stop=(kb == nb - 1)),
                             reads=[vb, pt], writes=[psO[c4]], self_sync=False)
                    P.op('pe', lambda e, pt=pt, kb=kb, nb=nb: e.matmul(psD[:], lhsT=K.ones_b[:], rhs=pt[:], start=(kb == 0), stop=(kb == nb - 1)),
                         reads=[K.ones_b, pt], writes=[psD], self_sync=False)
            psq = K.ps[7]
            P.op('pe', lambda e, h=h: e.matmul(psq[:], lhsT=selh[:, h, :], rhs=nFq.ap, start=True, stop=True), reads=[selh, NEG], writes=[psq], self_sync=False)
            qf, dn = K.f[0], K.f[1]
            P.op('act', lambda e, h=h: e.activation(out=qf[:], in_=psq[:], func=AF.Exp, scale=-1.0, bias=K.run[:, h:h + 1]), reads=[psq, K.run], writes=[qf])
            P.op('dve', lambda e: e.tensor_tensor(out=dn[:], in0=psD[:], in1=qf[:], op=ALU.mult), reads=[psD, qf], writes=[dn])
            P.op('act', lambda e: e.activation(out=dn[:], in_=dn[:], func=AF.Abs), reads=[dn], writes=[dn])
            P.op('dve', lambda e: e.tensor_scalar_max(out=dn[:], in0=dn[:], scalar1=1.0), reads=[dn], writes=[dn])
            P.op('dve', lambda e: e.reciprocal(out=dn[:], in_=dn[:]), reads=[dn], writes=[dn])
            P.op('dve', lambda e: e.tensor_tensor(out=dn[:], in0=dn[:], in1=qf[:], op=ALU.mult), reads=[dn, qf], writes=[dn])
            for c4 in range(4):
                P.op('dve', lambda e, c4=c4, h=h: e.tensor_tensor(out=K.B32[:, 4 * h + c4, :], in0=psO[c4][:], in1=dn[:], op=ALU.mult), reads=[psO[c4], dn], writes=[K.B32])
            ps_n = K.ps[7]
            for c4 in range(4):
                sq = K.sq[1]
                P.op('act', lambda e, c4=c4, h=h, sq=sq: e.activation(out=sq[:], in_=K.B32[:, 4 * h + c4, :], func=AF.Square), reads=[K.B32], writes=[sq])
                P.op('pe', lambda e, c4=c4, sq=sq: e.matmul(ps_n[:], lhsT=K.ones_f[:], rhs=sq[:], start=(c4 == 0), stop=(c4 == 3)), reads=[sq, K.ones_f], writes=[ps_n], self_sync=False)
            P.op('act', lambda e: e.activation(out=K.lnv[:], in_=ps_n[:], func=AF.Ln, scale=1.0 / 512, bias=K.eps_t[:, 0:1]), reads=[ps_n, K.eps_t], writes=[K.lnv])
            P.op('act', lambda e: e.activation(out=K.rs[:], in_=K.lnv[:], func=AF.Exp, scale=-0.5), reads=[K.lnv], writes=[K.rs])
            for c4 in range(4):
                m = 4 * h + c4
                P.op('dve', lambda e, m=m: e.scalar_tensor_tensor(out=K.B32[:, m, :], in0=K.B32[:, m, :], scalar=C.hg[:, m:m + 1], in1=K.rs[:], op0=ALU.mult, op1=ALU.mult),
                     reads=[K.B32, C.hg, K.rs], writes=[K.B32])
                P.op('pool', lambda e, m=m: e.tensor_tensor(out=K.H16[:, m, :], in0=K.B32[:, m, :], in1=SO[:, m, :], op=ALU.mult), reads=[K.B32, SO], writes=[K.H16])

        if globals().get('DEBUG_MIX') == 3:
            bf = K.B32.ap.rearrange("p a b -> p (a b)")
            P.op('dve', lambda e: e.tensor_copy(out=bf[:, 0:1024], in_=K.negc.ap.rearrange("p a b -> p (a b)")), reads=[K.negc], writes=[K.B32])
            P.op('dve', lambda e: e.tensor_copy(out=bf[:, 1024:1088], in_=K.bt[:]), reads=[K.bt], writes=[K.B32])
            P.op('dve', lambda e: e.tensor_copy(out=bf[:, 1088:1104], in_=K.run[:]), reads=[K.run], writes=[K.B32])
            P.op('dve', lambda e: e.tensor_copy(out=bf[:, 1104:1168], in_=K.sm[:]), reads=[K.sm], writes=[K.B32])
            K.store(C.xout, xview(C.xout)[:, :, i * T:(i + 1) * T], K.B32, K.B32[:])
            continue
        if globals().get('DEBUG_MIX') == 2:
            K.store(C.xout, xview(C.xout)[:, :, i * T:(i + 1) * T], K.B32, K.B32[:])
            continue

        def evo(m, ps):
            K.evac_copy(K.B32, K.B32[:, m, :], ps)
        K.linear_fm(lambda k: K.H16[:, k, :], KC, wo, wov, 0, 2048, evo, rhs_tks=[K.H16])
        finish_tile(K, C, li, i)


def layer_lru(K, C, li, NTILE):
    P = K.P
    w = C.w_in[li]
    wv_ = w.ap.rearrange("(k p) m -> p k m", p=128)
    wo = C.w_out[li]
    wov = wo.ap.rearrange("(c p) m -> p c m", p=LP)
    tb_ = P.sbuf([LP, 9, LC], F32, 'lrutab')
    for n_, (src, idx) in enumerate([(C.l_cw, 0), (C.l_cw, 1), (C.l_cw, 2), (C.l_cw, 3), (C.l_cb, None), (C.l_br, None), (C.l_bi, None), (C.l_lam, None)]):
        ap = src.ap[idx] if idx is not None else src.ap
        P.dma('sp', lambda e, n_=n_, ap=ap: e.dma_start(out=tb_[:, n_, :], in_=ap.rearrange("(c p) -> p c", p=LP), allow_slow_non_contiguous=True), reads=[src], writes=[tb_])
    P.op('act', lambda e: e.activation(out=tb_[:, 8, :], in_=tb_[:, 7, :], func=AF.Exp, scale=-1.0), reads=[tb_], writes=[tb_])
    P.op('act', lambda e: e.activation(out=tb_[:, 8, :], in_=tb_[:, 8, :], func=AF.Ln, bias=K.one_t[0:LP, 0:1]), reads=[tb_, K.one_t], writes=[tb_])
    P.op('dve', lambda e: e.tensor_scalar(out=tb_[:, 8, :], in0=tb_[:, 8, :], scalar1=-8.0, scalar2=None, op0=ALU.mult), reads=[tb_], writes=[tb_])
    hal = P.sbuf([LP, LC, 3], F32, 'hal')
    hst = P.sbuf([LP, LC], F32, 'hst')
    P.op('pool', lambda e: e.memset(hal[:], 0.0), writes=[hal])
    P.op('pool', lambda e: e.memset(hst[:], 0.0), writes=[hst])
    ub = [Tk(K.B32.ap.rearrange("p a b -> p (a b)")[0:LP, n_ * 516:n_ * 516 + 515]) for n_ in range(2)]
    ucf = [Tk(K.B32.ap.rearrange("p a b -> p (a b)")[0:LP, 2048 + n_ * 512:2048 + (n_ + 1) * 512]) for n_ in range(2)]
    ucb = [Tk(K.pT[n_].ap[0:LP, :]) for n_ in range(2)]
    wg = P.sbuf([LP, 2, 2, 168], BF16, 'wg')
    GG = [K.U[0], K.U[1]]
    YY = [K.U[2], K.U[3]]
    wr_v = C.l_wr.ap.rearrange("n (k p) e -> n p k e", p=LP)
    wi_v = C.l_wi.ap.rearrange("n (k p) e -> n p k e", p=LP)
    for i in range(NTILE):
        load_x_norm(K, C, li, i)

        def evg(m, ps):
            P.op('act', lambda e: e.activation(out=GG[m // 16][0:LP, m % 16, :], in_=ps[0:LP, :], func=AF.Gelu), reads=[ps], writes=[GG[m // 16]])
        K.linear_fm(lambda k: K.H16[:, k, :], KC, w, wv_, 0, LW, evg, mw=LP, rhs_tks=[K.H16])

        def evu(m, ps):
            n_ = m // 2
            c_ = m % 2
            u_ = ub[c_]
            B = K.B32
            P.op('dve', lambda e: e.tensor_copy(out=u_.ap[:, 0:3], in_=hal[:, m, :]), reads=[hal], writes=[B])
            P.op('act', lambda e: e.activation(out=u_.ap[:, 3:515], in_=ps[0:LP, :], func=AF.Copy), reads=[ps], writes=[B])
            P.op('dve', lambda e: e.tensor_copy(out=hal[:, m, :], in_=u_.ap[:, 512:515]), reads=[B], writes=[hal])
            uc = ucf[c_]
            P.op('dve', lambda e: e.tensor_scalar(out=uc.ap, in0=u_.ap[:, 0:512], scalar1=tb_[:, 0, m:m + 1], scalar2=tb_[:, 4, m:m + 1], op0=ALU.mult, op1=ALU.add),
                 reads=[B, tb_], writes=[B])
            for j in range(1, 4):
                P.op('dve', lambda e, j=j: e.scalar_tensor_tensor(out=uc.ap, in0=u_.ap[:, j:j + 512], scalar=tb_[:, j, m:m + 1], in1=uc.ap, op0=ALU.mult, op1=ALU.add),
                     reads=[B, tb_], writes=[B])
            P.op('pool', lambda e: e.tensor_copy(out=ucb[c_].ap, in_=uc.ap), reads=[B], writes=[K.pT[c_]])
            if c_ == 1:
                P.dma('pool', lambda e: e.dma_start(out=wg[:, 0, :, :], in_=wr_v[n_]), reads=[C.l_wr], writes=[wg])
                P.dma('pool', lambda e: e.dma_start(out=wg[:, 1, :, :], in_=wi_v[n_]), reads=[C.l_wi], writes=[wg])
                for mc in range(2):
                    ch = 2 * n_ + mc
                    psr = K.ps[6]
                    psi_ = K.ps[7]
                    for g_, pp in ((0, psr), (1, psi_)):
                        for kc in range(2):
                            P.op('pe', lambda e, g_=g_, pp=pp, kc=kc, mc=mc: e.matmul(pp[0:LP, :], lhsT=wg[:, g_, kc, mc * LP:(mc + 1) * LP], rhs=ucb[kc].ap, start=(kc == 0), stop=(kc == 1)),
                                 reads=[wg, K.pT[0], K.pT[1]], writes=[pp], self_sync=False)
                    a_ = Tk(K.sq[0].ap[0:LP, :])
                    s_ = Tk(K.sq[1].ap[0:LP, :])
                    g2_ = Tk(K.f[0].ap[0:LP, :])
                    P.op('act', lambda e, ch=ch: e.activation(out=a_.ap, in_=psr[0:LP, :], func=AF.Sigmoid, bias=tb_[:, 5, ch:ch + 1]), reads=[psr, tb_], writes=[K.sq[0]])
                    P.op('act', lambda e, ch=ch: e.activation(out=a_.ap, in_=a_.ap, func=AF.Exp, scale=tb_[:, 8, ch:ch + 1]), reads=[K.sq[0], tb_], writes=[K.sq[0]])
                    P.op('pool', lambda e: e.tensor_tensor(out=s_.ap, in0=a_.ap, in1=a_.ap, op=ALU.mult), reads=[K.sq[0]], writes=[K.sq[1]])
                    P.op('act', lambda e: e.activation(out=s_.ap, in_=s_.ap, func=AF.Sqrt, scale=-1.0, bias=K.one_t[0:LP, 0:1]), reads=[K.sq[1], K.one_t], writes=[K.sq[1]])
                    P.op('act', lambda e, ch=ch: e.activation(out=g2_.ap, in_=psi_[0:LP, :], func=AF.Sigmoid, bias=tb_[:, 6, ch:ch + 1]), reads=[psi_, tb_], writes=[K.f[0]])
                    P.op('dve', lambda e: e.tensor_tensor(out=s_.ap, in0=s_.ap, in1=g2_.ap, op=ALU.mult), reads=[K.sq[1], K.f[0]], writes=[K.sq[1]])
                    P.op('dve', lambda e, mc=mc: e.tensor_tensor(out=s_.ap, in0=s_.ap, in1=ucf[mc].ap, op=ALU.mult), reads=[K.sq[1], B], writes=[K.sq[1]])
                    P.op('dve', lambda e, ch=ch: e.tensor_tensor_scan(out=g2_.ap, data0=a_.ap, data1=s_.ap, initial=hst[:, ch:ch + 1], op0=ALU.mult, op1=ALU.add),
                         reads=[K.sq[0], K.sq[1], hst], writes=[K.f[0]])
                    P.op('dve', lambda e, ch=ch: e.tensor_copy(out=hst[:, ch:ch + 1], in_=g2_.ap[:, 511:512]), reads=[K.f[0]], writes=[hst])
                    P.op('pool', lambda e, ch=ch: e.tensor_tensor(out=YY[ch // 16][0:LP, ch % 16, :], in0=g2_.ap, in1=GG[ch // 16][0:LP, ch % 16, :], op=ALU.mult),
                         reads=[K.f[0], GG[ch // 16]], writes=[YY[ch // 16]])
        K.linear_fm(lambda k: K.H16[:, k, :], KC, w, wv_, LW, LW, evu, mw=LP, rhs_tks=[K.H16])

        def evo(m, ps):
            K.evac_copy(K.B32, K.B32[:, m, :], ps)
        K.linear_fm(lambda k: YY[k // 16][0:LP, k % 16, :], LC, wo, wov, 0, 2048, evo, kp=LP, rhs_tks=YY)
        finish_tile(K, C, li, i)


def build_program(nc, st, io, NTILE=16, layers=(0, 1, 2, 3), first_from_xin=True):
    K = Ker(nc, st)
    P = K.P
    alloc_small(K)
    C = Ctx()
    SS = NTILE * T
    C.layers = list(layers)
    C.xin = P.dram('x', [KC, 128, SS], F32, io('in'))
    C.xout = P.dram('y', [KC, 128, SS], F32, io('out'))
    gd = P.dram('norm_g', [128, 256], F32, io('in'))
    C.gt = P.sbuf([128, 256], F32, 'gt')
    K.load(C.gt, C.gt[:], gd, gd.ap)
    C.w1 = {}; C.w2 = {}; C.w_in = {}; C.w_out = {}
    for li in layers:
        C.w1[li] = P.dram('mlp_w1_%d' % li, [D, DFF], F32, io('in'))
        C.w2[li] = P.dram('mlp_w2_%d' % li, [DFF, D], F32, io('in'))
    if 0 in layers:
        C.w_in[0] = P.dram('fox_w_in', [D, 6160], F32, io('in'))
        C.w_out[0] = P.dram('fox_w_out', [D, D], F32, io('in'))
        C.fox_b = P.dram('fox_b_f', [16], F32, io('in'))
    if 1 in layers:
        C.w_in[1] = P.dram('lru_w_in', [D, 2 * LW], F32, io('in'))
        C.w_out[1] = P.dram('lru_w_out', [LW, D], F32, io('in'))
        C.l_cw = P.dram('lru_conv_w', [4, LW], F32, io('in'))
        C.l_cb = P.dram('lru_conv_b', [LW], F32, io('in'))
        C.l_wr = P.dram('lru_w_r', [16, 168, 168], F32, io('in'))
        C.l_br = P.dram('lru_b_r', [LW], F32, io('in'))
        C.l_wi = P.dram('lru_w_i', [16, 168, 168], F32, io('in'))
        C.l_bi = P.dram('lru_b_i', [LW], F32, io('in'))
        C.l_lam = P.dram('lru_lambda', [LW], F32, io('in'))
    if 2 in layers:
        C.w_in[2] = P.dram('sb_w_in', [D, 3 * D], F32, io('in'))
        C.w_out[2] = P.dram('sb_w_out', [D, D], F32, io('in'))
    if 3 in layers:
        C.w_in[3] = P.dram('mlstm_w_in', [D, 6152], F32, io('in'))
        C.w_out[3] = P.dram('mlstm_w_out', [D, D], F32, io('in'))
        C.ml_b = P.dram('mlstm_b_if', [8], F32, io('in'))
        hgd = P.dram('mlstm_head_g', [128, 16], F32, io('in'))
        C.hg = P.sbuf([128, 16], F32, 'hg')
        K.load(C.hg, C.hg[:], hgd, hgd.ap)
    C.kT = P.dram('kT_s', [16, 128, SS], BF16)
    C.vx = P.dram('vx_s', [16, 128, SS // 128, 128], BF16)
    C.vx4 = Tk(C.vx.ap.rearrange("(h c) p b d -> h p b (c d)", c=4) if False else C.vx.ap)
    for li in layers:
        if li == 0:
            layer_attn(K, C, 0, 'fox', NTILE)
        elif li == 1:
            layer_lru(K, C, 1, NTILE)
        elif li == 2:
            layer_attn(K, C, 2, 'sb', NTILE)
        else:
            vx_full = C.vx
            C.vx = P.dram('vx_m', [4, 128, SS // 128, 512], BF16)
            layer_mlstm(K, C, 3, NTILE)
            C.vx = vx_full
    P.finish([C.xout])
    P.emit()


def _io(kind):
    return "ExternalInput" if kind == 'in' else "ExternalOutput"


def gain_layout(g):
    return np.ascontiguousarray(g.reshape(KC, 128).T)


def make_in_map(inputs, xb, layers=(0, 1, 2, 3)):
    f = lambda a: np.ascontiguousarray(np.asarray(a, dtype=np.float32))
    ng = np.asarray(inputs['norm_g'])
    m = {'x': np.ascontiguousarray(xb.T.reshape(KC, 128, xb.shape[0])),
         'norm_g': np.ascontiguousarray(np.concatenate([gain_layout(ng[l, n]) for l in range(4) for n in range(4)], axis=1))}
    for li in layers:
        m['mlp_w1_%d' % li] = f(inputs['mlp_w1'][li])
        m['mlp_w2_%d' % li] = f(inputs['mlp_w2'][li])
    if 0 in layers:
        m['fox_w_in'] = f(inputs['fox_w_in'][0]); m['fox_w_out'] = f(inputs['fox_w_out'][0]); m['fox_b_f'] = f(inputs['fox_b_f'][0])
    if 1 in layers:
        for k_ in ('lru_w_in', 'lru_w_out', 'lru_conv_w', 'lru_conv_b', 'lru_w_r', 'lru_b_r', 'lru_w_i', 'lru_b_i', 'lru_lambda'):
            m[k_] = f(inputs[k_][0])
    if 2 in layers:
        m['sb_w_in'] = f(inputs['sb_w_in'][0]); m['sb_w_out'] = f(inputs['sb_w_out'][0])
    if 3 in layers:
        m['mlstm_w_in'] = f(inputs['mlstm_w_in'][0]); m['mlstm_w_out'] = f(inputs['mlstm_w_out'][0])
        m['mlstm_b_if'] = f(inputs['mlstm_b_if'][0]).reshape(8)
        m['mlstm_head_g'] = gain_layout(f(inputs['mlstm_head_g'][0]))
    return m


def run_layers(inputs, xs, layers, NTILE):
    nc = bass.Bass("TRN2", target_bir_lowering=False)
    with contextlib.ExitStack() as st:
        build_program(nc, st, _io, NTILE=NTILE, layers=layers)
    in_maps = [make_in_map(inputs, xb, layers) for xb in xs]
    res = run_bass_kernel_spmd(nc, in_maps, core_ids=list(range(len(in_maps))))
    return [r['y'].reshape(D, NTILE * T).T for r in res.results]


def kernel(**inputs):
    x = np.asarray(inputs['x'], dtype=np.float32)
    ys = run_layers(inputs, [x[0], x[1]], (0, 1, 2, 3), 16)
    return np.stack(ys, axis=0).astype(np.float32)
```

```python
import contextlib
import numpy as np
import ml_dtypes
import concourse.bass as bass
import concourse.mybir as mybir
from concourse.bass_utils import run_bass_kernel_spmd

F32 = mybir.dt.float32
BF16 = mybir.dt.bfloat16
AF = mybir.ActivationFunctionType
ALU = mybir.AluOpType
AX = mybir.AxisListType

D = 2048
KC = 16
NT = 2048
T = 512
NSL = 4
NCORE = 8
S = 8192
GT = 16
EPS = 1e-6
DFF = 8192
LW = 2688
LP = 84
LC = 32
ENGS = ('pe', 'dve', 'act', 'pool', 'sp')
NRING = {'sp': 12, 'pool': 12, 'act': 4}


class Tk:
    __slots__ = ('w', 'r', 'ap', 'name')

    def __init__(self, ap=None, name=None):
        self.w = None
        self.r = {}
        self.ap = ap
        self.name = name

    def __getitem__(self, idx):
        return self.ap[idx]


class Prog:
    def __init__(self, nc, stack):
        self.nc = nc
        self.stack = stack
        self.ops = {e: [] for e in ENGS}
        self.cnt = {e: 0 for e in ENGS}
        self.seen = {e: {} for e in ENGS}
        self.sems = {}
        for e in ENGS:
            self.sems[('E', e)] = stack.enter_context(nc.semaphore('s_' + e))
        self.ring = {}
        self.ring_cnt = {}
        self.ring_i = {}
        for q, n in NRING.items():
            self.ring[q] = []
            for i in range(n):
                key = ('D', q, i)
                self.sems[key] = stack.enter_context(nc.semaphore('d_%s%d' % (q, i)))
                self.ring[q].append(key)
            self.ring_cnt[q] = [0] * n
            self.ring_i[q] = 0
        self.n_tiles = 0

    def sbuf(self, shape, dt, name=None):
        self.n_tiles += 1
        name = name or 't%d' % self.n_tiles
        t = self.stack.enter_context(self.nc.sbuf_tensor(name, list(shape), dt))
        return Tk(t, name)

    def psum(self, shape, dt, name=None):
        self.n_tiles += 1
        name = name or 'p%d' % self.n_tiles
        t = self.stack.enter_context(self.nc.psum_tensor(name, list(shape), dt))
        return Tk(t, name)

    def dram(self, name, shape, dt, kind="Internal"):
        return Tk(self.nc.dram_tensor(name, list(shape), dt, kind=kind).ap(), name)

    def _waits(self, eng, reads, writes, self_sync):
        waits = {}
        seen = self.seen[eng]
        own = ('E', eng)

        def need(key, v):
            if (not self_sync) and key == own:
                return
            if seen.get(key, 0) >= v:
                return
            if waits.get(key, 0) < v:
                waits[key] = v

        for t in reads:
            if t.w is not None:
                need(*t.w)
        for t in writes:
            if t.w is not None:
                need(*t.w)
            for k, v in t.r.items():
                need(k, v)
        for k, v in waits.items():
            seen[k] = v
        return list(waits.items())

    def op(self, eng, fn, reads=(), writes=(), self_sync=True):
        waits = self._waits(eng, reads, writes, self_sync)
        self.cnt[eng] += 1
        n = self.cnt[eng]
        key = ('E', eng)
        self.ops[eng].append((waits, fn, key, 1))
        for t in reads:
            if t.r.get(key, 0) < n:
                t.r[key] = n
        for t in writes:
            t.w = (key, n)
            t.r = {}

    def dma(self, q, fn, reads=(), writes=()):
        i = self.ring_i[q]
        self.ring_i[q] = (i + 1) % len(self.ring[q])
        key = self.ring[q][i]
        waits = self._waits(q, reads, writes, True)
        prev = 16 * self.ring_cnt[q][i]
        if prev > 0 and self.seen[q].get(key, 0) < prev:
            waits.append((key, prev))
            self.seen[q][key] = prev
        self.ring_cnt[q][i] += 1
        v = 16 * self.ring_cnt[q][i]
        self.ops[q].append((waits, fn, key, 16))
        for t in reads:
            if t.r.get(key, 0) < v:
                t.r[key] = v
        for t in writes:
            t.w = (key, v)
            t.r = {}

    def finish(self, outs):
        waits = self._waits('sp', outs, (), True)
        self.ops['sp'].append((waits, None, None, 0))

    def emit(self):
        nc = self.nc
        sems = self.sems
        ops = self.ops

        def run(eng_name, eng):
            for waits, fn, key, inc in ops[eng_name]:
                for k, v in waits:
                    eng.wait_ge(sems[k], v)
                if fn is not None:
                    fn(eng).then_inc(sems[key], inc)

        with nc.Block() as block:
            @block.tensor
            def _(e):
                run('pe', e)

            @block.vector
            def _(e):
                run('dve', e)

            @block.scalar
            def _(e):
                run('act', e)

            @block.gpsimd
            def _(e):
                run('pool', e)

            @block.sync
            def _(e):
                run('sp', e)


class Ker:
    def __init__(self, nc, st):
        self.nc = nc
        P = self.P = Prog(nc, st)
        self.A32 = P.sbuf([128, KC, T], F32, 'A32')
        self.B32 = P.sbuf([128, KC, T], F32, 'B32')
        self.H16 = P.sbuf([128, KC, T], BF16, 'H16')
        self.U = [P.sbuf([128, 16, T], BF16, 'U%d' % i) for i in range(4)]
        self.W = [P.sbuf([128, 8192], BF16, 'W%d' % i) for i in range(2)]
        self.wi = 0
        self.sq = [P.sbuf([128, T], F32, 'sq%d' % i) for i in range(2)]
        self.sqi = 0
        self.rs = P.sbuf([128, T], F32, 'rs')
        self.pT = [P.sbuf([128, T], BF16, 'pT%d' % i) for i in range(2)]
        self.pti = 0
        self.f = [P.sbuf([128, T], F32, 'f%d' % i) for i in range(2)]
        self.lnv = self.f[1]
        self.ps = [P.psum([128, T], F32, 'ps%d' % i) for i in range(8)]
        self.psi = 0
        self.evi = 0
        self.ones_f = P.sbuf([128, 128], F32, 'ones_f')
        self.ones_b = P.sbuf([128, 128], BF16, 'ones_b')
        self.tri_f = P.sbuf([128, 128], F32, 'tri_f')
        self.triu_f = P.sbuf([128, 128], F32, 'triu_f')
        self.eps_t = P.sbuf([128, 1], F32, 'eps_t')
        self.one_t = P.sbuf([128, 1], F32, 'one_t')
        P.op('pool', lambda e: e.memset(self.ones_f[:], 1.0), writes=[self.ones_f])
        P.op('pool', lambda e: e.memset(self.ones_b[:], 1.0), writes=[self.ones_b])
        P.op('pool', lambda e: e.memset(self.eps_t[:], EPS), writes=[self.eps_t])
        P.op('pool', lambda e: e.memset(self.one_t[:], 1.0), writes=[self.one_t])
        P.op('pool', lambda e: e.memset(self.tri_f[:], 1.0), writes=[self.tri_f])
        P.op('pool', lambda e: e.affine_select(out=self.tri_f[:], in_=self.tri_f[:], pattern=[[1, 128]],
                                               compare_op=ALU.is_ge, fill=0.0, base=0, channel_multiplier=-1),
             reads=[self.tri_f], writes=[self.tri_f])
        P.op('pool', lambda e: e.memset(self.triu_f[:], 1.0), writes=[self.triu_f])
        P.op('pool', lambda e: e.affine_select(out=self.triu_f[:], in_=self.triu_f[:], pattern=[[-1, 128]],
                                               compare_op=ALU.is_ge, fill=0.0, base=0, channel_multiplier=1),
             reads=[self.triu_f], writes=[self.triu_f])

    def wbuf(self):
        w = self.W[self.wi]
        self.wi = (self.wi + 1) % 2
        return w

    def psA(self):
        p = self.ps[self.psi]
        self.psi = (self.psi + 1) % 2
        return p

    def ptile(self):
        p = self.pT[self.pti]
        self.pti = (self.pti + 1) % 2
        return p

    def load(self, dst, dst_ap, src_tk, src_ap, q='sp'):
        self.P.dma(q, lambda e: e.dma_start(out=dst_ap, in_=src_ap), reads=[src_tk], writes=[dst])

    def store(self, dst_tk, dst_ap, src, src_ap, q='sp'):
        self.P.dma(q, lambda e: e.dma_start(out=dst_ap, in_=src_ap), reads=[src], writes=[dst_tk])

    def evac_copy(self, dst, dst_ap, ps, ps_ap=None, scale=None):
        P = self.P
        ps_ap = ps[:] if ps_ap is None else ps_ap
        self.evi += 1
        if scale is not None:
            P.op('act', lambda e: e.mul(out=dst_ap, in_=ps_ap, mul=scale), reads=[ps], writes=[dst])
        elif self.evi % 2:
            P.op('act', lambda e: e.activation(out=dst_ap, in_=ps_ap, func=AF.Copy), reads=[ps], writes=[dst])
        else:
            P.op('dve', lambda e: e.tensor_copy(out=dst_ap, in_=ps_ap), reads=[ps], writes=[dst])

    def rstd(self, src, nch, dim, ps=None):
        P = self.P
        ps = ps or self.ps[7]
        for k in range(nch):
            sq = self.sq[self.sqi]
            self.sqi ^= 1
            P.op('act', lambda e, k=k, sq=sq: e.activation(out=sq[:], in_=src[:, k, :], func=AF.Square), reads=[src], writes=[sq])
            P.op('pe', lambda e, k=k, sq=sq: e.matmul(ps[:], lhsT=self.ones_f[:], rhs=sq[:], start=(k == 0), stop=(k == nch - 1)),
                 reads=[sq, self.ones_f], writes=[ps], self_sync=False)
        P.op('act', lambda e: e.activation(out=self.lnv[:], in_=ps[:], func=AF.Ln, scale=1.0 / dim, bias=self.eps_t[:, 0:1]),
             reads=[ps, self.eps_t], writes=[self.lnv])
        P.op('act', lambda e: e.activation(out=self.rs[:], in_=self.lnv[:], func=AF.Exp, scale=-0.5), reads=[self.lnv], writes=[self.rs])
        return self.rs

    def norm_apply(self, dst, src, g, nch, rs, dst_off=0, g_off=0):
        P = self.P
        for k in range(nch):
            P.op('dve', lambda e, k=k: e.scalar_tensor_tensor(out=dst[:, dst_off + k, :], in0=src[:, k, :],
                                                              scalar=g[:, g_off + k:g_off + k + 1], in1=rs[:],
                                                              op0=ALU.mult, op1=ALU.mult),
                 reads=[src, g, rs], writes=[dst])

    def linear_fm(self, rhs_of, nk, w_tk, w_view, col0, ncols, evac, kp=128, mw=128, rhs_tks=()):
        P = self.P
        gw = (min(512, 8192 // nk) // mw) * mw
        m_idx = 0
        c = 0
        while c < ncols:
            w = min(gw, ncols - c)
            wb = self.wbuf()
            wv = wb.ap[0:kp, 0:nk * w].rearrange("p (k m) -> p k m", m=w)
            P.dma('pool', lambda e, wv=wv, c=c, w=w: e.dma_start(out=wv, in_=w_view[:, :, col0 + c:col0 + c + w]),
                  reads=[w_tk], writes=[wb])
            for j in range(w // mw):
                ps = self.psA()
                for k in range(nk):
                    P.op('pe', lambda e, wv=wv, j=j, k=k, ps=ps: e.matmul(ps[0:mw, :], lhsT=wv[:, k, j * mw:(j + 1) * mw], rhs=rhs_of(k),
                                                                         start=(k == 0), stop=(k == nk - 1)),
                         reads=[wb] + list(rhs_tks), writes=[ps], self_sync=False)
                evac(m_idx, ps)
                m_idx += 1
            c += w


def xview(tk):
    return tk.ap.rearrange("k p t -> p k t")


def wview(wb, nk, w, kp=128):
    return wb.ap[0:kp, 0:nk * w].rearrange("p (k m) -> p k m", m=w)


def residual_norm_add(K, x, y, g, g_off):
    P = K.P
    rs = K.rstd(y, KC, D)
    K.norm_apply(y, y, g, KC, rs, g_off=g_off)
    for k in range(KC):
        P.op('dve', lambda e, k=k: e.tensor_tensor(out=x[:, k, :], in0=x[:, k, :], in1=y[:, k, :], op=ALU.add), reads=[x, y], writes=[x])


def mlp_tile(K, xm, gt, goff, w1, w2, out_tk, out_ap):
    P = K.P
    rs = K.rstd(xm, KC, D)
    K.norm_apply(K.H16, xm, gt, KC, rs, g_off=goff)
    w1v = w1.ap.rearrange("(k p) m -> p k m", p=128)

    def ev1(m, ps):
        u = K.U[m // 16]
        f = K.f[0]
        P.op('act', lambda e: e.activation(out=f[:], in_=ps[:], func=AF.Relu), reads=[ps], writes=[f])
        eng = 'dve'
        P.op(eng, lambda e: e.tensor_tensor(out=u[:, m % 16, :], in0=f[:], in1=f[:], op=ALU.mult), reads=[f], writes=[u])

    K.linear_fm(lambda k: K.H16[:, k, :], KC, w1, w1v, 0, DFF, ev1, rhs_tks=[K.H16])
    w2v = w2.ap.rearrange("(q k p) m -> q p k m", q=4, p=128)
    for cg in range(4):
        acc = K.ps[2:6]
        for kq in range(4):
            wb = K.wbuf()
            wv = wview(wb, 16, 512)
            P.dma('pool', lambda e, wv=wv, kq=kq, cg=cg: e.dma_start(out=wv, in_=w2v[kq][:, :, cg * 512:(cg + 1) * 512]),
                  reads=[w2], writes=[wb])
            for j in range(4):
                for k in range(16):
                    P.op('pe', lambda e, wv=wv, j=j, k=k, kq=kq: e.matmul(acc[j][:], lhsT=wv[:, k, j * 128:(j + 1) * 128], rhs=K.U[kq][:, k, :],
                                                                         start=(kq == 0 and k == 0), stop=(kq == 3 and k == 15)),
                         reads=[wb, K.U[kq]], writes=[acc[j]], self_sync=False)
        for j in range(4):
            K.evac_copy(K.B32, K.B32[:, cg * 4 + j, :], acc[j])
    residual_norm_add(K, xm, K.B32, gt, goff + KC)
    K.store(out_tk, out_ap, xm, xm[:])


def proj_tm(K, w_tk, w_view, col0, ncols, emit):
    P = K.P
    for cg in range(ncols // 512):
        wb = K.wbuf()
        wv = wview(wb, 16, 512)
        P.dma('pool', lambda e, wv=wv, cg=cg: e.dma_start(out=wv, in_=w_view[:, :, col0 + cg * 512:col0 + (cg + 1) * 512]),
              reads=[w_tk], writes=[wb])
        for tb in range(4):
            ps = K.psA()
            for k in range(16):
                P.op('pe', lambda e, wv=wv, tb=tb, k=k, ps=ps: e.matmul(ps[:], lhsT=K.H16[:, k, tb * 128:(tb + 1) * 128], rhs=wv[:, k, :],
                                                                       start=(k == 0), stop=(k == 15)),
                     reads=[wb, K.H16], writes=[ps], self_sync=False)
            emit(cg, tb, ps)


def small_tm(K, wsm, ncol, tb, ps):
    P = K.P
    for k in range(16):
        P.op('pe', lambda e, k=k: e.matmul(ps[:, 0:ncol], lhsT=K.H16[:, k, tb * 128:(tb + 1) * 128], rhs=wsm[:, k, 0:ncol],
                                           start=(k == 0), stop=(k == 15)),
             reads=[wsm, K.H16], writes=[ps], self_sync=False)


def softplus_neg(K, dst, dst_ap, src, src_ap, bias_ap, bias_tk, tmp, tmp_ap):
    P = K.P
    P.op('dve', lambda e: e.tensor_tensor(out=tmp_ap, in0=src_ap, in1=bias_ap, op=ALU.add), reads=[src, bias_tk], writes=[tmp])
    P.op('act', lambda e: e.activation(out=tmp_ap, in_=tmp_ap, func=AF.Exp, scale=-1.0), reads=[tmp], writes=[tmp])
    P.op('act', lambda e: e.activation(out=dst_ap, in_=tmp_ap, func=AF.Ln, bias=K.one_t[:, 0:1]), reads=[tmp, K.one_t], writes=[dst])


def cumsum_block(K, dst, dst_ap, sp, sp_ap, run, ncol, ps):
    P = K.P
    P.op('pe', lambda e: e.matmul(ps[:, 0:ncol], lhsT=K.tri_f[:], rhs=sp_ap, start=True, stop=True), reads=[K.tri_f, sp], writes=[ps], self_sync=False)
    P.op('dve', lambda e: e.tensor_tensor(out=dst_ap, in0=ps[:, 0:ncol], in1=run[:, 0:ncol], op=ALU.add), reads=[ps, run], writes=[dst])
    P.op('pe', lambda e: e.matmul(ps[:, 64:64 + ncol], lhsT=K.ones_f[:], rhs=sp_ap, start=True, stop=True), reads=[K.ones_f, sp], writes=[ps], self_sync=False)
    P.op('dve', lambda e: e.tensor_tensor(out=run[:, 0:ncol], in0=run[:, 0:ncol], in1=ps[:, 64:64 + ncol], op=ALU.add), reads=[ps, run], writes=[run])


class Ctx:
    pass


def alloc_small(K):
    P = K.P
    K.negc = P.sbuf([128, 64, 16], F32, 'negc')
    K.run = P.sbuf([128, 16], F32, 'run')
    K.bt = P.sbuf([128, 64], F32, 'bt')
    K.cm = P.sbuf([128, 4, 513], BF16, 'cm')
    K.sm = P.sbuf([128, 64], F32, 'sm')
    K.wsm = P.sbuf([128, 16, 16], BF16, 'wsm')
    K.bb = P.sbuf([128, 16], F32, 'bb')
    P.op('pool', lambda e: e.memset(K.cm[:], 1.0), writes=[K.cm])
    for j in range(4):
        P.op('pool', lambda e, j=j: e.affine_select(out=K.cm[:, j, :], in_=K.cm[:, j, :], pattern=[[1, 513]], compare_op=ALU.is_ge, fill=0.0,
                                                    base=-1 - 128 * j, channel_multiplier=-1), reads=[K.cm], writes=[K.cm])


def load_x_norm(K, C, li, i):
    src = C.xin if li == C.layers[0] else C.xout
    K.load(K.A32, K.A32[:], src, xview(src)[:, :, i * T:(i + 1) * T])
    rs = K.rstd(K.A32, KC, D)
    K.norm_apply(K.H16, K.A32, C.gt, KC, rs, g_off=li * 64)


def finish_tile(K, C, li, i):
    if globals().get('DEBUG_MIX'):
        K.store(C.xout, xview(C.xout)[:, :, i * T:(i + 1) * T], K.B32, K.B32[:])
        return
    residual_norm_add(K, K.A32, K.B32, C.gt, li * 64 + 16)
    mlp_tile(K, K.A32, C.gt, li * 64 + 32, C.w1[li], C.w2[li], C.xout, xview(C.xout)[:, :, i * T:(i + 1) * T])


def kv_proj(K, C, w, wv_, i, qcol, kcol, vcol, nq, qscale, kscale, vh, vd):
    P = K.P

    def evq(m, ps):
        K.evac_copy(K.U[3], K.U[3][:, m, :], ps, scale=qscale)
    K.linear_fm(lambda k: K.H16[:, k, :], KC, w, wv_, qcol, nq * 128, evq, rhs_tks=[K.H16])

    def evk(m, ps):
        st_ = K.ptile()
        K.evac_copy(st_, st_[:], ps, scale=kscale)
        K.store(C.kT, C.kT.ap[m, :, i * T:(i + 1) * T], st_, st_[:])
    K.linear_fm(lambda k: K.H16[:, k, :], KC, w, wv_, kcol, nq * 128, evk, rhs_tks=[K.H16])

    hpg = 512 // vd

    def evv(cg, tb, ps):
        st_ = K.ptile()
        K.evac_copy(st_, st_[:], ps)
        if hpg >= 1:
            K.store(C.vx, C.vx.ap[cg * hpg:(cg + 1) * hpg, :, i * 4 + tb, :].rearrange("h p d -> p h d"), st_, st_[:].rearrange("p (h d) -> p h d", d=vd))
    if vd <= 512:
        proj_tm(K, w, wv_, vcol, 2048, evv)


def layer_attn(K, C, li, kind, NTILE):
    P = K.P
    w = C.w_in[li]
    wv_ = w.ap.rearrange("(k p) m -> p k m", p=128)
    wo = C.w_out[li]
    wov = wo.ap.rearrange("(k p) m -> p k m", p=128)
    scale = 128 ** -0.5
    if kind == 'fox':
        K.load(K.wsm, K.wsm[:], w, wv_[:, :, 6144:6160], q='pool')
        K.load(K.bb, K.bb[:], C.fox_b, C.fox_b.ap.partition_broadcast(128))
        P.op('pool', lambda e: e.memset(K.run[:], 0.0), writes=[K.run])
    OT = K.U[2]
    for i in range(NTILE):
        load_x_norm(K, C, li, i)
        kv_proj(K, C, w, wv_, i, 0, 2048, 4096, 16, scale, None, 16, 128)
        nb = 4 * (i + 1)
        if kind == 'fox':
            for tb in range(4):
                ps = K.ps[6]
                small_tm(K, K.wsm, 16, tb, ps)
                softplus_neg(K, K.sm, K.sm[:, 0:16], ps, ps[:, 0:16], K.bb[:], K.bb, K.sm, K.sm[:, 16:32])
                cumsum_block(K, K.negc, K.negc[:, i * 4 + tb, :], K.sm, K.sm[:, 0:16], K.run, 16, K.ps[7])
        for h in range(16):
            if h % 2 == 0:
                KTH, VH = K.U[0], K.U[1]
                kth = KTH.ap.rearrange("p a b -> p (a b)")
                vh = VH.ap.rearrange("p a b -> p (a b)")
            else:
                KTH, VH = K.W[0], K.W[1]
                kth = KTH.ap
                vh = VH.ap
            K.load(KTH, kth[:, 0:nb * 128], C.kT, C.kT.ap[h, :, 0:nb * 128])
            K.load(VH, vh[:, 0:nb * 128].rearrange("p (a b) -> p a b", b=128), C.vx, C.vx.ap[h, :, 0:nb, :])
            psO, psD = K.ps[2], K.ps[3]
            if kind == 'fox':
                P.op('dve', lambda e, h=h, nb=nb: e.tensor_scalar(out=K.bt[:, 0:nb], in0=K.negc[:, 0:nb, h], scalar1=K.run[:, h:h + 1], scalar2=0.0,
                                                                 op0=ALU.subtract, op1=ALU.min), reads=[K.negc, K.run], writes=[K.bt])
                for kb in range(nb):
                    ps = K.psA()
                    P.op('pe', lambda e, kb=kb, h=h, ps=ps, kth=kth: e.matmul(ps[:], lhsT=kth[:, kb * 128:(kb + 1) * 128], rhs=K.U[3][:, h, :], start=True, stop=True),
                         reads=[KTH, K.U[3]], writes=[ps], self_sync=False)
                    pt = K.ptile()
                    P.op('act', lambda e, kb=kb, ps=ps, pt=pt: e.activation(out=pt[:], in_=ps[:], func=AF.Exp, bias=K.bt[:, kb:kb + 1]),
                         reads=[ps, K.bt], writes=[pt])
                    if kb >= 4 * i:
                        j = kb - 4 * i
                        P.op('dve', lambda e, j=j, pt=pt: e.tensor_tensor(out=pt[:], in0=pt[:], in1=K.cm[:, j, 1:513], op=ALU.mult), reads=[pt, K.cm], writes=[pt])
                    P.op('pe', lambda e, kb=kb, pt=pt, nb=nb, vh=vh: e.matmul(psO[:], lhsT=vh[:, kb * 128:(kb + 1) * 128], rhs=pt[:], start=(kb == 0), stop=(kb == nb - 1)),
                         reads=[VH, pt], writes=[psO], self_sync=False)
                    P.op('pe', lambda e, kb=kb, pt=pt, nb=nb: e.matmul(psD[:], lhsT=K.ones_b[:], rhs=pt[:], start=(kb == 0), stop=(kb == nb - 1)),
                         reads=[K.ones_b, pt], writes=[psD], self_sync=False)
                rd = K.f[0]
                P.op('dve', lambda e: e.reciprocal(out=rd[:], in_=psD[:]), reads=[psD], writes=[rd])
                P.op('dve', lambda e, h=h: e.tensor_tensor(out=OT[:, h, :], in0=psO[:], in1=rd[:], op=ALU.mult), reads=[psO, rd], writes=[OT])
            else:
                spsum = K.sq[0]
                e1, sp, ex2 = K.f[0], K.f[1], K.sq[1]
                P.op('dve', lambda e: e.memset(spsum[:], 0.0), writes=[spsum])
                for n_, kb in enumerate(range(nb - 1, -1, -1)):
                    ps = K.psA()
                    P.op('pe', lambda e, kb=kb, h=h, ps=ps, kth=kth: e.matmul(ps[:], lhsT=kth[:, kb * 128:(kb + 1) * 128], rhs=K.U[3][:, h, :], start=True, stop=True),
                         reads=[KTH, K.U[3]], writes=[ps], self_sync=False)
                    P.op('act', lambda e, ps=ps: e.activation(out=e1[:], in_=ps[:], func=AF.Exp), reads=[ps], writes=[e1])
                    P.op('act', lambda e: e.activation(out=sp[:], in_=e1[:], func=AF.Ln, bias=K.one_t[:, 0:1]), reads=[e1, K.one_t], writes=[sp])
                    if kb >= 4 * i:
                        j = kb - 4 * i
                        P.op('dve', lambda e, j=j: e.tensor_tensor(out=sp[:], in0=sp[:], in1=K.cm[:, j, 0:512], op=ALU.mult), reads=[sp, K.cm], writes=[sp])
                    psE = K.ps[6 + n_ % 2]
                    P.op('pe', lambda e, psE=psE: e.matmul(psE[:], lhsT=K.triu_f[:], rhs=sp[:], start=True, stop=False), reads=[K.triu_f, sp], writes=[psE], self_sync=False)
                    P.op('pe', lambda e, psE=psE: e.matmul(psE[:], lhsT=K.ones_f[:], rhs=spsum[:], start=False, stop=True), reads=[K.ones_f, spsum], writes=[psE], self_sync=False)
                    P.op('act', lambda e, psE=psE: e.activation(out=ex2[:], in_=psE[:], func=AF.Exp, scale=-1.0), reads=[psE], writes=[ex2])
                    pt = K.ptile()
                    P.op('dve', lambda e, pt=pt: e.tensor_tensor(out=pt[:], in0=e1[:], in1=ex2[:], op=ALU.mult), reads=[e1, ex2], writes=[pt])
                    if kb >= 4 * i:
                        j = kb - 4 * i
                        P.op('dve', lambda e, j=j, pt=pt: e.tensor_tensor(out=pt[:], in0=pt[:], in1=K.cm[:, j, 0:512], op=ALU.mult), reads=[pt, K.cm], writes=[pt])
                    P.op('dve', lambda e: e.tensor_tensor(out=spsum[:], in0=spsum[:], in1=sp[:], op=ALU.add), reads=[spsum, sp], writes=[spsum])
                    P.op('pe', lambda e, kb=kb, pt=pt, n_=n_, nb=nb, vh=vh: e.matmul(psO[:], lhsT=vh[:, kb * 128:(kb + 1) * 128], rhs=pt[:], start=(n_ == 0), stop=(n_ == nb - 1)),
                         reads=[VH, pt], writes=[psO], self_sync=False)
                K.evac_copy(OT, OT[:, h, :], psO)

        def evo(m, ps):
            K.evac_copy(K.B32, K.B32[:, m, :], ps)
        K.linear_fm(lambda k: OT[:, k, :], KC, wo, wov, 0, 2048, evo, rhs_tks=[OT])
        finish_tile(K, C, li, i)


def layer_mlstm(K, C, li, NTILE):
    P = K.P
    w = C.w_in[li]
    wv_ = w.ap.rearrange("(k p) m -> p k m", p=128)
    wo = C.w_out[li]
    wov = wo.ap.rearrange("(k p) m -> p k m", p=128)
    K.load(K.wsm, K.wsm[:, :, 0:8], w, wv_[:, :, 6144:6152], q='pool')
    K.load(K.bb, K.bb[:, 0:8], C.ml_b, C.ml_b.ap.partition_broadcast(128))
    P.op('pool', lambda e: e.memset(K.run[:], 0.0), writes=[K.run])
    nF = Tk(K.negc.ap.rearrange("p a b -> p (a b)")[:, 0:256].rearrange("p (a b) -> p a b", b=4))
    ik = Tk(K.negc.ap.rearrange("p a b -> p (a b)")[:, 256:512].rearrange("p (a b) -> p a b", b=4))
    nFq = Tk(K.negc.ap.rearrange("p a b -> p (a b)")[0:8, 512:1024])
    carry = Tk(K.sm.ap[0:8, 32:33])
    bcol = Tk(K.sm.ap[0:8, 33:34])
    onesr = Tk(K.rs.ap[0:8, :])
    selh = P.sbuf([8, 4, 128], F32, 'selh')
    NEG = K.negc
    P.op('pool', lambda e: e.memset(carry.ap, 0.0), writes=[K.sm])
    P.op('pool', lambda e: e.memset(selh[:], 1.0), writes=[selh])
    for h in range(4):
        P.op('pool', lambda e, h=h: e.affine_select(out=selh[:, h, :], in_=selh[:, h, :], pattern=[[0, 128]], compare_op=ALU.is_equal, fill=0.0,
                                                    base=-(4 + h), channel_multiplier=1), reads=[selh], writes=[selh])
    K.load(K.sm, bcol.ap, C.ml_b, C.ml_b.ap.rearrange("(a b) -> a b", b=1))
    K0, K1, SO = K.U[0], K.U[1], K.U[2]
    k0 = K0.ap.rearrange("p a b -> p (a b)")
    k1 = K1.ap.rearrange("p a b -> p (a b)")
    for i in range(NTILE):
        load_x_norm(K, C, li, i)
        kv_proj(K, C, w, wv_, i, 0, 1024, 2048, 8, None, 256 ** -0.5, 4, 512)

        def evs(m, ps):
            P.op('act', lambda e: e.activation(out=SO[:, m, :], in_=ps[:], func=AF.Sigmoid), reads=[ps], writes=[SO])
        K.linear_fm(lambda k: K.H16[:, k, :], KC, w, wv_, 4096, 2048, evs, rhs_tks=[K.H16])
        nb = 4 * (i + 1)
        for tb in range(4):
            ps = K.ps[6]
            small_tm(K, K.wsm, 8, tb, ps)
            P.op('dve', lambda e, tb=tb, i=i, ps=ps: e.tensor_tensor(out=ik[:, i * 4 + tb, :], in0=ps[:, 0:4], in1=K.bb[:, 0:4], op=ALU.add), reads=[ps, K.bb], writes=[NEG])
            softplus_neg(K, K.sm, K.sm[:, 0:4], ps, ps[:, 4:8], K.bb[:, 4:8], K.bb, K.sm, K.sm[:, 16:20])
            cumsum_block(K, NEG, nF[:, i * 4 + tb, :], K.sm, K.sm[:, 0:4], K.run, 4, K.ps[7])
        psg = K.ps[6]
        for k in range(16):
            P.op('pe', lambda e, k=k: e.matmul(psg[0:8, :], lhsT=K.wsm[:, k, 0:8], rhs=K.H16[:, k, :], start=(k == 0), stop=(k == 15)),
                 reads=[K.wsm, K.H16], writes=[psg], self_sync=False)
        gq = Tk(K.sq[0].ap[0:8, :])
        P.op('dve', lambda e: e.tensor_scalar_add(out=gq.ap, in0=psg[0:8, :], scalar1=bcol.ap), reads=[psg, K.sm], writes=[K.sq[0]])
        P.op('dve', lambda e: e.memset(onesr.ap, 1.0), writes=[K.rs])
        P.op('act', lambda e: e.activation(out=gq.ap, in_=gq.ap, func=AF.Exp, scale=-1.0), reads=[K.sq[0]], writes=[K.sq[0]])
        P.op('act', lambda e: e.activation(out=gq.ap, in_=gq.ap, func=AF.Ln, bias=K.one_t[0:8, 0:1]), reads=[K.sq[0], K.one_t], writes=[K.sq[0]])
        P.op('dve', lambda e: e.tensor_tensor_scan(out=nFq.ap, data0=onesr.ap, data1=gq.ap, initial=carry.ap, op0=ALU.mult, op1=ALU.add),
             reads=[K.sq[0], K.rs, K.sm], writes=[NEG])
        P.op('dve', lambda e: e.tensor_copy(out=carry.ap, in_=nFq.ap[:, 511:512]), reads=[NEG], writes=[K.sm])
        for h in range(4):
            K.load(K0, k0[:, 0:nb * 128], C.kT, C.kT.ap[2 * h, :, 0:nb * 128])
            K.load(K1, k1[:, 0:nb * 128], C.kT, C.kT.ap[2 * h + 1, :, 0:nb * 128])
            P.op('dve', lambda e, h=h, nb=nb: e.tensor_scalar(out=K.bt[:, 0:nb], in0=nF[:, 0:nb, h], scalar1=K.run[:, h:h + 1], scalar2=0.0,
                                                             op0=ALU.subtract, op1=ALU.min), reads=[NEG, K.run], writes=[K.bt])
            P.op('dve', lambda e, h=h, nb=nb: e.tensor_tensor(out=K.bt[:, 0:nb], in0=K.bt[:, 0:nb], in1=ik[:, 0:nb, h], op=ALU.add), reads=[NEG, K.bt], writes=[K.bt])
            P.op('act', lambda e, nb=nb: e.activation(out=K.bt[:, 0:nb], in_=K.bt[:, 0:nb], func=AF.Exp), reads=[K.bt], writes=[K.bt])
            psO = K.ps[2:6]
            psD = K.ps[6]
            for kt in range(i + 1):
                vb = K.wbuf()
                vv = vb.ap[:, 0:2048].rearrange("p (a b) -> p a b", b=512)
                K.load(vb, vv, C.vx, C.vx.ap[h, :, kt * 4:(kt + 1) * 4, :])
                for j in range(4):
                    kb = kt * 4 + j
                    ps = K.psA()
                    P.op('pe', lambda e, kb=kb, h=h, ps=ps: e.matmul(ps[:], lhsT=k0[:, kb * 128:(kb + 1) * 128], rhs=K.U[3][:, 2 * h, :], start=True, stop=False),
                         reads=[K0, K.U[3]], writes=[ps], self_sync=False)
                    P.op('pe', lambda e, kb=kb, h=h, ps=ps: e.matmul(ps[:], lhsT=k1[:, kb * 128:(kb + 1) * 128], rhs=K.U[3][:, 2 * h + 1, :], start=False, stop=True),
                         reads=[K1, K.U[3]], writes=[ps], self_sync=False)
                    pt = K.ptile()
                    P.op('dve', lambda e, kb=kb, ps=ps, pt=pt: e.tensor_scalar_mul(out=pt[:], in0=ps[:], scalar1=K.bt[:, kb:kb + 1]), reads=[ps, K.bt], writes=[pt])
                    if kt == i:
                        P.op('dve', lambda e, j=j, pt=pt: e.tensor_tensor(out=pt[:], in0=pt[:], in1=K.cm[:, j, 1:513], op=ALU.mult), reads=[pt, K.cm], writes=[pt])
                    for c4 in range(4):
                        P.op('pe', lambda e, j=j, c4=c4, pt=pt, kb=kb, vv=vv, nb=nb: e.matmul(psO[c4][:], lhsT=vv[:, j, c4 * 128:(c4 + 1) * 128], rhs=pt[:], start=(kb == 0), stop=(kb == nb - 1)),
                             reads=[vb, pt], writes=[psO[c4]], self_sync=False)
                    P.op('pe', lambda e, pt=pt, kb=kb, nb=nb: e.matmul(psD[:], lhsT=K.ones_b[:], rhs=pt[:], start=(kb == 0), stop=(kb == nb - 1)),
                         reads=[K.ones_b, pt], writes=[psD], self_sync=False)
            psq = K.ps[7]
            P.op('pe', lambda e, h=h: e.matmul(psq[:], lhsT=selh[:, h, :], rhs=nFq.ap, start=True, stop=True), reads=[selh, NEG], writes=[psq], self_sync=False)
            qf, dn = K.f[0], K.f[1]
            P.op('act', lambda e, h=h: e.activation(out=qf[:], in_=psq[:], func=AF.Exp, scale=-1.0, bias=K.run[:, h:h + 1]), reads=[psq, K.run], writes=[qf])
            P.op('dve', lambda e: e.tensor_tensor(out=dn[:], in0=psD[:], in1=qf[:], op=ALU.mult), reads=[psD, qf], writes=[dn])
            P.op('act', lambda e: e.activation(out=dn[:], in_=dn[:], func=AF.Abs), reads=[dn], writes=[dn])
            P.op('dve', lambda e: e.tensor_scalar_max(out=dn[:], in0=dn[:], scalar1=1.0), reads=[dn], writes=[dn])
            P.op('dve', lambda e: e.reciprocal(out=dn[:], in_=dn[:]), reads=[dn], writes=[dn])
            P.op('dve', lambda e: e.tensor_tensor(out=dn[:], in0=dn[:], in1=qf[:], op=ALU.mult), reads=[dn, qf], writes=[dn])
            for c4 in range(4):
                P.op('dve', lambda e, c4=c4, h=h: e.tensor_tensor(out=K.B32[:, 4 * h + c4, :], in0=psO[c4][:], in1=dn[:], op=ALU.mult), reads=[psO[c4], dn], writes=[K.B32])
            ps_n = K.ps[7]
            for c4 in range(4):
                sq = K.sq[1]
                P.op('act', lambda e, c4=c4, h=h, sq=sq: e.activation(out=sq[:], in_=K.B32[:, 4 * h + c4, :], func=AF.Square), reads=[K.B32], writes=[sq])
                P.op('pe', lambda e, c4=c4, sq=sq: e.matmul(ps_n[:], lhsT=K.ones_f[:], rhs=sq[:], start=(c4 == 0), stop=(c4 == 3)), reads=[sq, K.ones_f], writes=[ps_n], self_sync=False)
            P.op('act', lambda e: e.activation(out=K.lnv[:], in_=ps_n[:], func=AF.Ln, scale=1.0 / 512, bias=K.eps_t[:, 0:1]), reads=[ps_n, K.eps_t], writes=[K.lnv])
            P.op('act', lambda e: e.activation(out=K.rs[:], in_=K.lnv[:], func=AF.Exp, scale=-0.5), reads=[K.lnv], writes=[K.rs])
            for c4 in range(4):
                m = 4 * h + c4
                P.op('dve', lambda e, m=m: e.scalar_tensor_tensor(out=K.B32[:, m, :], in0=K.B32[:, m, :], scalar=C.hg[:, m:m + 1], in1=K.rs[:], op0=ALU.mult, op1=ALU.mult),
                     reads=[K.B32, C.hg, K.rs], writes=[K.B32])
                P.op('dve', lambda e, m=m: e.tensor_tensor(out=K.H16[:, m, :], in0=K.B32[:, m, :], in1=SO[:, m, :], op=ALU.mult), reads=[K.B32, SO], writes=[K.H16])

        if globals().get('DEBUG_MIX') == 3:
            bf = K.B32.ap.rearrange("p a b -> p (a b)")
            P.op('dve', lambda e: e.tensor_copy(out=bf[:, 0:1024], in_=K.negc.ap.rearrange("p a b -> p (a b)")), reads=[K.negc], writes=[K.B32])
            P.op('dve', lambda e: e.tensor_copy(out=bf[:, 1024:1088], in_=K.bt[:]), reads=[K.bt], writes=[K.B32])
            P.op('dve', lambda e: e.tensor_copy(out=bf[:, 1088:1104], in_=K.run[:]), reads=[K.run], writes=[K.B32])
            P.op('dve', lambda e: e.tensor_copy(out=bf[:, 1104:1168], in_=K.sm[:]), reads=[K.sm], writes=[K.B32])
            K.store(C.xout, xview(C.xout)[:, :, i * T:(i + 1) * T], K.B32, K.B32[:])
            continue
        if globals().get('DEBUG_MIX') == 2:
            K.store(C.xout, xview(C.xout)[:, :, i * T:(i + 1) * T], K.B32, K.B32[:])
            continue

        def evo(m, ps):
            K.evac_copy(K.B32, K.B32[:, m, :], ps)
        K.linear_fm(lambda k: K.H16[:, k, :], KC, wo, wov, 0, 2048, evo, rhs_tks=[K.H16])
        finish_tile(K, C, li, i)


def layer_lru(K, C, li, NTILE):
    P = K.P
    w = C.w_in[li]
    wv_ = w.ap.rearrange("(k p) m -> p k m", p=128)
    wo = C.w_out[li]
    wov = wo.ap.rearrange("(c p) m -> p c m", p=LP)
    tb_ = P.sbuf([LP, 9, LC], F32, 'lrutab')
    for n_, (src, idx) in enumerate([(C.l_cw, 0), (C.l_cw, 1), (C.l_cw, 2), (C.l_cw, 3), (C.l_cb, None), (C.l_br, None), (C.l_bi, None), (C.l_lam, None)]):
        ap = src.ap[idx] if idx is not None else src.ap
        P.dma('sp', lambda e, n_=n_, ap=ap: e.dma_start(out=tb_[:, n_, :], in_=ap.rearrange("(c p) -> p c", p=LP), allow_slow_non_contiguous=True), reads=[src], writes=[tb_])
    P.op('act', lambda e: e.activation(out=tb_[:, 8, :], in_=tb_[:, 7, :], func=AF.Exp, scale=-1.0), reads=[tb_], writes=[tb_])
    P.op('act', lambda e: e.activation(out=tb_[:, 8, :], in_=tb_[:, 8, :], func=AF.Ln, bias=K.one_t[0:LP, 0:1]), reads=[tb_, K.one_t], writes=[tb_])
    P.op('dve', lambda e: e.tensor_scalar(out=tb_[:, 8, :], in0=tb_[:, 8, :], scalar1=-8.0, scalar2=None, op0=ALU.mult), reads=[tb_], writes=[tb_])
    hal = P.sbuf([LP, LC, 3], F32, 'hal')
    hst = P.sbuf([LP, LC], F32, 'hst')
    P.op('pool', lambda e: e.memset(hal[:], 0.0), writes=[hal])
    P.op('pool', lambda e: e.memset(hst[:], 0.0), writes=[hst])
    ub = [Tk(K.B32.ap.rearrange("p a b -> p (a b)")[0:LP, n_ * 516:n_ * 516 + 515]) for n_ in range(2)]
    ucf = [Tk(K.B32.ap.rearrange("p a b -> p (a b)")[0:LP, 2048 + n_ * 512:2048 + (n_ + 1) * 512]) for n_ in range(2)]
    ucb = [Tk(K.pT[n_].ap[0:LP, :]) for n_ in range(2)]
    wg = P.sbuf([LP, 2, 2, 168], BF16, 'wg')
    GG = [K.U[0], K.U[1]]
    YY = [K.U[2], K.U[3]]
    wr_v = C.l_wr.ap.rearrange("n (k p) e -> n p k e", p=LP)
    wi_v = C.l_wi.ap.rearrange("n (k p) e -> n p k e", p=LP)
    for i in range(NTILE):
        load_x_norm(K, C, li, i)

        def evg(m, ps):
            P.op('act', lambda e: e.activation(out=GG[m // 16][0:LP, m % 16, :], in_=ps[0:LP, :], func=AF.Gelu), reads=[ps], writes=[GG[m // 16]])
        K.linear_fm(lambda k: K.H16[:, k, :], KC, w, wv_, 0, LW, evg, mw=LP, rhs_tks=[K.H16])

        def evu(m, ps):
            n_ = m // 2
            c_ = m % 2
            u_ = ub[c_]
            B = K.B32
            P.op('dve', lambda e: e.tensor_copy(out=u_.ap[:, 0:3], in_=hal[:, m, :]), reads=[hal], writes=[B])
            P.op('act', lambda e: e.activation(out=u_.ap[:, 3:515], in_=ps[0:LP, :], func=AF.Copy), reads=[ps], writes=[B])
            P.op('dve', lambda e: e.tensor_copy(out=hal[:, m, :], in_=u_.ap[:, 512:515]), reads=[B], writes=[hal])
            uc = ucf[c_]
            P.op('dve', lambda e: e.tensor_scalar(out=uc.ap, in0=u_.ap[:, 0:512], scalar1=tb_[:, 0, m:m + 1], scalar2=tb_[:, 4, m:m + 1], op0=ALU.mult, op1=ALU.add),
                 reads=[B, tb_], writes=[B])
            for j in range(1, 4):
                P.op('dve', lambda e, j=j: e.scalar_tensor_tensor(out=uc.ap, in0=u_.ap[:, j:j + 512], scalar=tb_[:, j, m:m + 1], in1=uc.ap, op0=ALU.mult, op1=ALU.add),
                     reads=[B, tb_], writes=[B])
            P.op('dve', lambda e: e.tensor_copy(out=ucb[c_].ap, in_=uc.ap), reads=[B], writes=[K.pT[c_]])
            if c_ == 1:
                P.dma('pool', lambda e: e.dma_start(out=wg[:, 0, :, :], in_=wr_v[n_]), reads=[C.l_wr], writes=[wg])
                P.dma('pool', lambda e: e.dma_start(out=wg[:, 1, :, :], in_=wi_v[n_]), reads=[C.l_wi], writes=[wg])
                for mc in range(2):
                    ch = 2 * n_ + mc
                    psr = K.ps[6]
                    psi_ = K.ps[7]
                    for g_, pp in ((0, psr), (1, psi_)):
                        for kc in range(2):
                            P.op('pe', lambda e, g_=g_, pp=pp, kc=kc, mc=mc: e.matmul(pp[0:LP, :], lhsT=wg[:, g_, kc, mc * LP:(mc + 1) * LP], rhs=ucb[kc].ap, start=(kc == 0), stop=(kc == 1)),
                                 reads=[wg, K.pT[0], K.pT[1]], writes=[pp], self_sync=False)
                    a_ = Tk(K.sq[0].ap[0:LP, :])
                    s_ = Tk(K.sq[1].ap[0:LP, :])
                    g2_ = Tk(K.f[0].ap[0:LP, :])
                    P.op('act', lambda e, ch=ch: e.activation(out=a_.ap, in_=psr[0:LP, :], func=AF.Sigmoid, bias=tb_[:, 5, ch:ch + 1]), reads=[psr, tb_], writes=[K.sq[0]])
                    P.op('act', lambda e, ch=ch: e.activation(out=a_.ap, in_=a_.ap, func=AF.Exp, scale=tb_[:, 8, ch:ch + 1]), reads=[K.sq[0], tb_], writes=[K.sq[0]])
                    P.op('dve', lambda e: e.tensor_tensor(out=s_.ap, in0=a_.ap, in1=a_.ap, op=ALU.mult), reads=[K.sq[0]], writes=[K.sq[1]])
                    P.op('act', lambda e: e.activation(out=s_.ap, in_=s_.ap, func=AF.Sqrt, scale=-1.0, bias=K.one_t[0:LP, 0:1]), reads=[K.sq[1], K.one_t], writes=[K.sq[1]])
                    P.op('act', lambda e, ch=ch: e.activation(out=g2_.ap, in_=psi_[0:LP, :], func=AF.Sigmoid, bias=tb_[:, 6, ch:ch + 1]), reads=[psi_, tb_], writes=[K.f[0]])
                    P.op('dve', lambda e: e.tensor_tensor(out=s_.ap, in0=s_.ap, in1=g2_.ap, op=ALU.mult), reads=[K.sq[1], K.f[0]], writes=[K.sq[1]])
                    P.op('dve', lambda e, mc=mc: e.tensor_tensor(out=s_.ap, in0=s_.ap, in1=ucf[mc].ap, op=ALU.mult), reads=[K.sq[1], B], writes=[K.sq[1]])
                    P.op('dve', lambda e, ch=ch: e.tensor_tensor_scan(out=g2_.ap, data0=a_.ap, data1=s_.ap, initial=hst[:, ch:ch + 1], op0=ALU.mult, op1=ALU.add),
                         reads=[K.sq[0], K.sq[1], hst], writes=[K.f[0]])
                    P.op('dve', lambda e, ch=ch: e.tensor_copy(out=hst[:, ch:ch + 1], in_=g2_.ap[:, 511:512]), reads=[K.f[0]], writes=[hst])
                    P.op('dve', lambda e, ch=ch: e.tensor_tensor(out=YY[ch // 16][0:LP, ch % 16, :], in0=g2_.ap, in1=GG[ch // 16][0:LP, ch % 16, :], op=ALU.mult),
                         reads=[K.f[0], GG[ch // 16]], writes=[YY[ch // 16]])
        K.linear_fm(lambda k: K.H16[:, k, :], KC, w, wv_, LW, LW, evu, mw=LP, rhs_tks=[K.H16])

        def evo(m, ps):
            K.evac_copy(K.B32, K.B32[:, m, :], ps)
        K.linear_fm(lambda k: YY[k // 16][0:LP, k % 16, :], LC, wo, wov, 0, 2048, evo, kp=LP, rhs_tks=YY)
        finish_tile(K, C, li, i)


def build_program(nc, st, io, NTILE=16, layers=(0, 1, 2, 3), first_from_xin=True):
    K = Ker(nc, st)
    P = K.P
    alloc_small(K)
    C = Ctx()
    SS = NTILE * T
    C.layers = list(layers)
    C.xin = P.dram('x', [KC, 128, SS], F32, io('in'))
    C.xout = P.dram('y', [KC, 128, SS], F32, io('out'))
    gd = P.dram('norm_g', [128, 256], F32, io('in'))
    C.gt = P.sbuf([128, 256], F32, 'gt')
    K.load(C.gt, C.gt[:], gd, gd.ap)
    C.w1 = {}; C.w2 = {}; C.w_in = {}; C.w_out = {}
    for li in layers:
        C.w1[li] = P.dram('mlp_w1_%d' % li, [D, DFF], F32, io('in'))
        C.w2[li] = P.dram('mlp_w2_%d' % li, [DFF, D], F32, io('in'))
    if 0 in layers:
        C.w_in[0] = P.dram('fox_w_in', [D, 6160], F32, io('in'))
        C.w_out[0] = P.dram('fox_w_out', [D, D], F32, io('in'))
        C.fox_b = P.dram('fox_b_f', [16], F32, io('in'))
    if 1 in layers:
        C.w_in[1] = P.dram('lru_w_in', [D, 2 * LW], F32, io('in'))
        C.w_out[1] = P.dram('lru_w_out', [LW, D], F32, io('in'))
        C.l_cw = P.dram('lru_conv_w', [4, LW], F32, io('in'))
        C.l_cb = P.dram('lru_conv_b', [LW], F32, io('in'))
        C.l_wr = P.dram('lru_w_r', [16, 168, 168], F32, io('in'))
        C.l_br = P.dram('lru_b_r', [LW], F32, io('in'))
        C.l_wi = P.dram('lru_w_i', [16, 168, 168], F32, io('in'))
        C.l_bi = P.dram('lru_b_i', [LW], F32, io('in'))
        C.l_lam = P.dram('lru_lambda', [LW], F32, io('in'))
    if 2 in layers:
        C.w_in[2] = P.dram('sb_w_in', [D, 3 * D], F32, io('in'))
        C.w_out[2] = P.dram('sb_w_out', [D, D], F32, io('in'))
    if 3 in layers:
        C.w_in[3] = P.dram('mlstm_w_in', [D, 6152], F32, io('in'))
        C.w_out[3] = P.dram('mlstm_w_out', [D, D], F32, io('in'))
        C.ml_b = P.dram('mlstm_b_if', [8], F32, io('in'))
        hgd = P.dram('mlstm_head_g', [128, 16], F32, io('in'))
        C.hg = P.sbuf([128, 16], F32, 'hg')
        K.load(C.hg, C.hg[:], hgd, hgd.ap)
    def to_bf16(tk, nsplit=16):
        shp = list(tk.ap.shape)
        tb = P.dram(tk.name + '_bf', shp, BF16)
        rows = shp[0] // nsplit
        for r in range(nsplit):
            P.dma('pool', lambda e, r=r: e.dma_start(out=tb.ap[r * rows:(r + 1) * rows], in_=tk.ap[r * rows:(r + 1) * rows]), reads=[tk], writes=[tb])
        return tb
    for li in layers:
        C.w_in[li] = to_bf16(C.w_in[li])
        C.w_out[li] = to_bf16(C.w_out[li])
    for li in layers:
        C.w1[li] = to_bf16(C.w1[li])
        C.w2[li] = to_bf16(C.w2[li])
    C.kT = P.dram('kT_s', [16, 128, SS], BF16)
    C.vx = P.dram('vx_s', [16, 128, SS // 128, 128], BF16)
    C.vx4 = Tk(C.vx.ap.rearrange("(h c) p b d -> h p b (c d)", c=4) if False else C.vx.ap)
    for li in layers:
        if li == 0:
            layer_attn(K, C, 0, 'fox', NTILE)
        elif li == 1:
            layer_lru(K, C, 1, NTILE)
        elif li == 2:
            layer_attn(K, C, 2, 'sb', NTILE)
        else:
            vx_full = C.vx
            C.vx = P.dram('vx_m', [4, 128, SS // 128, 512], BF16)
            layer_mlstm(K, C, 3, NTILE)
            C.vx = vx_full
    P.finish([C.xout])
    P.emit()


def _io(kind):
    return "ExternalInput" if kind == 'in' else "ExternalOutput"


def gain_layout(g):
    return np.ascontiguousarray(g.reshape(KC, 128).T)


def make_in_map(inputs, xb, layers=(0, 1, 2, 3)):
    f = lambda a: np.ascontiguousarray(np.asarray(a, dtype=np.float32))
    ng = np.asarray(inputs['norm_g'])
    m = {'x': np.ascontiguousarray(xb.T.reshape(KC, 128, xb.shape[0])),
         'norm_g': np.ascontiguousarray(np.concatenate([gain_layout(ng[l, n]) for l in range(4) for n in range(4)], axis=1))}
    for li in layers:
        m['mlp_w1_%d' % li] = f(inputs['mlp_w1'][li])
        m['mlp_w2_%d' % li] = f(inputs['mlp_w2'][li])
    if 0 in layers:
        m['fox_w_in'] = f(inputs['fox_w_in'][0]); m['fox_w_out'] = f(inputs['fox_w_out'][0]); m['fox_b_f'] = f(inputs['fox_b_f'][0])
    if 1 in layers:
        for k_ in ('lru_w_in', 'lru_w_out', 'lru_conv_w', 'lru_conv_b', 'lru_w_r', 'lru_b_r', 'lru_w_i', 'lru_b_i', 'lru_lambda'):
            m[k_] = f(inputs[k_][0])
    if 2 in layers:
        m['sb_w_in'] = f(inputs['sb_w_in'][0]); m['sb_w_out'] = f(inputs['sb_w_out'][0])
    if 3 in layers:
        m['mlstm_w_in'] = f(inputs['mlstm_w_in'][0]); m['mlstm_w_out'] = f(inputs['mlstm_w_out'][0])
        m['mlstm_b_if'] = f(inputs['mlstm_b_if'][0]).reshape(8)
        m['mlstm_head_g'] = gain_layout(f(inputs['mlstm_head_g'][0]))
    return m


def run_layers(inputs, xs, layers, NTILE):
    nc = bass.Bass("TRN2", target_bir_lowering=False)
    with contextlib.ExitStack() as st:
        build_program(nc, st, _io, NTILE=NTILE, layers=layers)
    in_maps = [make_in_map(inputs, xb, layers) for xb in xs]
    res = run_bass_kernel_spmd(nc, in_maps, core_ids=list(range(len(in_maps))))
    return [r['y'].reshape(D, NTILE * T).T for r in res.results]


def kernel(**inputs):
    x = np.asarray(inputs['x'], dtype=np.float32)
    ys = run_layers(inputs, [x[0], x[1]], (0, 1, 2, 3), 16)
    return np.stack(ys, axis=0).astype(np.float32)
```
